# Optimizing a Trainium2 kernel written in Bass

```python
import math
import jax, jax.numpy as jnp
from jax import lax
import numpy as np

D_MODEL = 1024
BATCH = 16
SEQ = 2048
DEPTH = 1

N_HEADS_ATTN = 8
HEAD_DIM = 64
V_DIM = 2 * HEAD_DIM
ATTN_WIDTH = N_HEADS_ATTN * V_DIM
QK_WIDTH = N_HEADS_ATTN * 2 * HEAD_DIM
Q_BLOCK = 128
REL_BUCKETS = 32
REL_MAX_DIST = 128
LRU_WIDTH = D_MODEL
LRU_BLOCKS = 8
LRU_BLOCK = LRU_WIDTH // LRU_BLOCKS
CONV_WIDTH = 4
LRU_C = 8.0
N_EXPERTS = 16
EC_CAPACITY_FACTOR = 2
D_FF_EXPERT = 2 * D_MODEL
EPS = 1e-6

OFF_Q = 0
OFF_K = OFF_Q + QK_WIDTH
OFF_V = OFF_K + QK_WIDTH
OFF_LRU_X = OFF_V + ATTN_WIDTH
OFF_LRU_Y = OFF_LRU_X + LRU_WIDTH
OFF_GATE_A = OFF_LRU_Y + LRU_WIDTH
OFF_GATE_R = OFF_GATE_A + D_MODEL
D_IN = OFF_GATE_R + D_MODEL

kernel_name = "hybrid_diffattn_rglru_ecmoe_encoder"


def _rmsnorm(x, g):
    xf = x.astype(jnp.float32)
    y = xf * lax.rsqrt(jnp.mean(xf * xf, axis=-1, keepdims=True) + EPS)
    return (y * g.astype(jnp.float32)).astype(x.dtype)


def _lambda_init(layer_idx):
    return 0.8 - 0.6 * math.exp(-0.3 * layer_idx)


def _rel_bucket(rel):
    half = REL_BUCKETS // 2
    max_exact = half // 2
    ret = jnp.where(rel > 0, half, 0)
    n = jnp.abs(rel)
    nf = jnp.maximum(n, max_exact).astype(jnp.float32)
    large = max_exact + (jnp.log(nf / max_exact) / math.log(REL_MAX_DIST / max_exact)
                         * (half - max_exact)).astype(jnp.int32)
    large = jnp.minimum(large, half - 1)
    return ret + jnp.where(n < max_exact, n, large)


def _diff_attention(q, k, v, lam, rel_bias):
    B, S = q.shape[0], q.shape[1]
    nq = S // Q_BLOCK
    qb = q.reshape(B, nq, Q_BLOCK, N_HEADS_ATTN, 2, HEAD_DIM).transpose(1, 0, 3, 4, 2, 5)
    kt = k.transpose(0, 2, 3, 1, 4)
    vt = v.transpose(0, 2, 1, 3)
    k_pos = jnp.arange(S, dtype=jnp.int32)
    scale = HEAD_DIM ** -0.5

    def block(args):
        q_blk, start = args
        q_pos = start + jnp.arange(Q_BLOCK, dtype=jnp.int32)
        bias = rel_bias[_rel_bucket(k_pos[None, :] - q_pos[:, None])]
        bias = bias.transpose(2, 0, 1).astype(jnp.float32)
        logits = jnp.einsum("bhmqd,bhmkd->bhmqk", q_blk, kt).astype(jnp.float32) * scale
        p = jax.nn.softmax(logits + bias[None, :, None], axis=-1)
        w = (p[:, :, 0] - lam * p[:, :, 1]).astype(v.dtype)
        return jnp.einsum("bhqk,bhkv->bhqv", w, vt)

    starts = jnp.arange(nq, dtype=jnp.int32) * Q_BLOCK
    o = lax.map(block, (qb, starts))
    return o.transpose(1, 0, 3, 2, 4).reshape(B, S, N_HEADS_ATTN, V_DIM)


def _scan_combine(c1, c2):
    a1, b1 = c1
    a2, b2 = c2
    return a1 * a2, a2 * b1 + b2


def _rg_lru(xf, w_r, b_r, w_i, b_i, lam_param, reverse):
    B, S, W = xf.shape
    xb = xf.reshape(B, S, LRU_BLOCKS, LRU_BLOCK)
    r = jax.nn.sigmoid(jnp.einsum("bsnc,ncd->bsnd", xb, w_r.astype(jnp.float32)).reshape(B, S, W)
                       + b_r.astype(jnp.float32))
    i = jax.nn.sigmoid(jnp.einsum("bsnc,ncd->bsnd", xb, w_i.astype(jnp.float32)).reshape(B, S, W)
                       + b_i.astype(jnp.float32))
    log_a = -LRU_C * r * jax.nn.softplus(-lam_param.astype(jnp.float32))
    a = jnp.exp(log_a)
    mult = jnp.sqrt(jnp.maximum(-jnp.expm1(2.0 * log_a), 0.0))
    _, h = lax.associative_scan(_scan_combine, (a, mult * i * xf), axis=1, reverse=reverse)
    return h


def _ec_moe(xn, w_router, w_gate, w_up, w_down):
    B, S, D = xn.shape
    cap = EC_CAPACITY_FACTOR * S // N_EXPERTS
    aff = jax.nn.softmax(jnp.einsum("bsd,de->bse", xn.astype(jnp.float32),
                                    w_router.astype(jnp.float32)), axis=-1)
    vals, idx = lax.top_k(aff.transpose(0, 2, 1), cap)
    xg = jax.vmap(lambda xs, ix: xs[ix])(xn, idx)
    hg = jax.nn.silu(jnp.einsum("becd,edf->becf", xg, w_gate)) * jnp.einsum("becd,edf->becf", xg, w_up)
    ye = jnp.einsum("becf,efd->becd", hg, w_down) * vals[..., None].astype(xn.dtype)
    return jax.vmap(lambda ix, yb: jnp.zeros((S, D), yb.dtype).at[ix.reshape(-1)].add(yb.reshape(-1, D)))(idx, ye)


def setup_inputs(seed: int = 0) -> dict:
    key = jax.random.key(seed)
    ks = jax.random.split(key, 32)
    f32 = jnp.float32
    nrm = lambda k, shape, s: jax.random.normal(k, shape, f32) * s
    u = jax.random.uniform(ks[20], (DEPTH, 2, LRU_WIDTH), f32, 0.9, 0.999)
    return {
        "x": jax.random.normal(ks[0], (BATCH, SEQ, D_MODEL), f32),
        "g_mix": 1.0 + nrm(ks[1], (DEPTH, D_MODEL), 0.05),
        "w_in": nrm(ks[2], (DEPTH, D_MODEL, D_IN), D_MODEL ** -0.5),
        "g_q": 1.0 + nrm(ks[3], (DEPTH, HEAD_DIM), 0.05),
        "g_k": 1.0 + nrm(ks[4], (DEPTH, HEAD_DIM), 0.05),
        "lam_q1": nrm(ks[5], (DEPTH, HEAD_DIM), 0.1),
        "lam_k1": nrm(ks[6], (DEPTH, HEAD_DIM), 0.1),
        "lam_q2": nrm(ks[7], (DEPTH, HEAD_DIM), 0.1),
        "lam_k2": nrm(ks[8], (DEPTH, HEAD_DIM), 0.1),
        "g_subln": 1.0 + nrm(ks[9], (DEPTH, V_DIM), 0.05),
        "rel_bias": nrm(ks[10], (REL_BUCKETS, N_HEADS_ATTN), 0.5),
        "conv_w": nrm(ks[11], (DEPTH, CONV_WIDTH, LRU_WIDTH), CONV_WIDTH ** -0.5),
        "conv_b": nrm(ks[12], (DEPTH, LRU_WIDTH), 0.01),
        "gate_r_w": nrm(ks[13], (DEPTH, 2, LRU_BLOCKS, LRU_BLOCK, LRU_BLOCK), LRU_BLOCK ** -0.5),
        "gate_r_b": nrm(ks[14], (DEPTH, 2, LRU_WIDTH), 0.01),
        "gate_i_w": nrm(ks[15], (DEPTH, 2, LRU_BLOCKS, LRU_BLOCK, LRU_BLOCK), LRU_BLOCK ** -0.5),
        "gate_i_b": nrm(ks[16], (DEPTH, 2, LRU_WIDTH), 0.01),
        "lru_lambda": jnp.log(u) - jnp.log1p(-u),
        "w_proj_attn": nrm(ks[17], (DEPTH, ATTN_WIDTH, D_MODEL), ATTN_WIDTH ** -0.5),
        "w_proj_lru": nrm(ks[18], (DEPTH, LRU_WIDTH, D_MODEL), LRU_WIDTH ** -0.5),
        "w_out": nrm(ks[19], (DEPTH, D_MODEL, D_MODEL), D_MODEL ** -0.5),
        "g_ffn": 1.0 + nrm(ks[21], (DEPTH, D_MODEL), 0.05),
        "w_router": nrm(ks[22], (DEPTH, D_MODEL, N_EXPERTS), D_MODEL ** -0.5),
        "w_gate_e": nrm(ks[23], (DEPTH, N_EXPERTS, D_MODEL, D_FF_EXPERT), D_MODEL ** -0.5),
        "w_up_e": nrm(ks[24], (DEPTH, N_EXPERTS, D_MODEL, D_FF_EXPERT), D_MODEL ** -0.5),
        "w_down_e": nrm(ks[25], (DEPTH, N_EXPERTS, D_FF_EXPERT, D_MODEL), D_FF_EXPERT ** -0.5),
    }


def reference(x, g_mix, w_in, g_q, g_k, lam_q1, lam_k1, lam_q2, lam_k2, g_subln, rel_bias,
              conv_w, conv_b, gate_r_w, gate_r_b, gate_i_w, gate_i_b, lru_lambda,
              w_proj_attn, w_proj_lru, w_out, g_ffn, w_router, w_gate_e, w_up_e, w_down_e):
    B, S, D = x.shape
    h = x
    for layer in range(DEPTH):
        lam_init = _lambda_init(layer)
        xn = _rmsnorm(h, g_mix[layer])
        proj = jnp.einsum("bsd,dn->bsn", xn, w_in[layer])
        q = proj[..., OFF_Q:OFF_K].reshape(B, S, N_HEADS_ATTN, 2, HEAD_DIM)
        k = proj[..., OFF_K:OFF_V].reshape(B, S, N_HEADS_ATTN, 2, HEAD_DIM)
        v = proj[..., OFF_V:OFF_LRU_X].reshape(B, S, N_HEADS_ATTN, V_DIM)
        x_lru = proj[..., OFF_LRU_X:OFF_LRU_Y]
        y_lru = proj[..., OFF_LRU_Y:OFF_GATE_A]
        gate_a = proj[..., OFF_GATE_A:OFF_GATE_R]
        gate_r = proj[..., OFF_GATE_R:D_IN]

        q = _rmsnorm(q, g_q[layer])
        k = _rmsnorm(k, g_k[layer])
        lam = (jnp.exp(jnp.sum(lam_q1[layer].astype(jnp.float32) * lam_k1[layer].astype(jnp.float32)))
               - jnp.exp(jnp.sum(lam_q2[layer].astype(jnp.float32) * lam_k2[layer].astype(jnp.float32)))
               + lam_init)
        o = _diff_attention(q, k, v, lam, rel_bias)
        o = _rmsnorm(o, g_subln[layer]) * (1.0 - lam_init)
        branch_a = jnp.einsum("bsa,ad->bsd", o.reshape(B, S, ATTN_WIDTH), w_proj_attn[layer])

        xc = lax.conv_general_dilated(
            x_lru, conv_w[layer][:, None, :], window_strides=(1,),
            padding=[(CONV_WIDTH // 2, CONV_WIDTH - 1 - CONV_WIDTH // 2)],
            dimension_numbers=("NWC", "WIO", "NWC"), feature_group_count=LRU_WIDTH) + conv_b[layer]
        xcf = xc.astype(jnp.float32)
        h_fwd = _rg_lru(xcf, gate_r_w[layer, 0], gate_r_b[layer, 0], gate_i_w[layer, 0],
                        gate_i_b[layer, 0], lru_lambda[layer, 0], reverse=False)
        h_bwd = _rg_lru(xcf, gate_r_w[layer, 1], gate_r_b[layer, 1], gate_i_w[layer, 1],
                        gate_i_b[layer, 1], lru_lambda[layer, 1], reverse=True)
        lru_out = ((h_fwd + h_bwd) * jax.nn.gelu(y_lru.astype(jnp.float32))).astype(x.dtype)
        branch_r = jnp.einsum("bsw,wd->bsd", lru_out, w_proj_lru[layer])

        mixed = jax.nn.sigmoid(gate_a) * branch_a + jax.nn.sigmoid(gate_r) * branch_r
        h = h + jnp.einsum("bsd,de->bse", mixed, w_out[layer])

        hn = _rmsnorm(h, g_ffn[layer])
        h = h + _ec_moe(hn, w_router[layer], w_gate_e[layer], w_up_e[layer], w_down_e[layer])
    return h
```

```python
import math
import numpy as np
from contextlib import ExitStack
import concourse.bass as bass
import concourse.mybir as mybir
from concourse.bass_utils import run_bass_kernel_spmd

F32 = mybir.dt.float32
BF16 = mybir.dt.bfloat16
U32 = mybir.dt.uint32
I32 = mybir.dt.int32
ALU = mybir.AluOpType
AF = mybir.ActivationFunctionType

NCORES = 8
NSEQ = 2
S = 2048
D = 1024
DIN = 7168
NH = 8
NE = 16
CAP = 256
DFF = 2048
EPS = 1e-6
LAM_INIT = 0.8 - 0.6 * math.exp(-0.3 * 0)
STRIP_W = 1152
NEG_BIG = -1.0e30

SAME_ENGINE_SYNC = True


class Res:
    __slots__ = ("name", "w", "r")

    def __init__(self, name=""):
        self.name = name
        self.w = None
        self.r = {}


class Eng:
    def __init__(self, name, h, sem):
        self.name, self.h, self.sem = name, h, sem
        self.count = 0
        self.waited = {}

    def wait_ev(self, ev):
        if ev is None:
            return
        sem, val = ev
        if sem is self.sem and (not SAME_ENGINE_SYNC or self.name == "pe"):
            return
        if self.waited.get(id(sem), 0) >= val:
            return
        self.h.wait_ge(sem, val)
        self.waited[id(sem)] = val

    def deps(self, reads, writes):
        for r in reads:
            self.wait_ev(r.w)
        for w in writes:
            self.wait_ev(w.w)
            for ev in list(w.r.values()):
                self.wait_ev(ev)

    def op(self, fns, reads=(), writes=()):
        self.deps(reads, writes)
        if not isinstance(fns, (list, tuple)):
            fns = [fns]
        ins = None
        for f in fns:
            ins = f(self.h)
        self.count += 1
        ins.then_inc(self.sem, 1)
        ev = (self.sem, self.count)
        for r in reads:
            r.r[id(self.sem)] = ev
        for w in writes:
            w.w = ev
            w.r = {}
        return ev


class DmaQ:
    def __init__(self, name, eng, sems):
        self.name, self.eng, self.sems = name, eng, sems
        self.n = 0
        self.last = {}

    def dma(self, fn, reads=(), writes=()):
        k = len(self.sems)
        sem = self.sems[self.n % k]
        val = 16 * (self.n // k + 1)
        self.eng.deps(reads, writes)
        if val > 16:
            self.eng.wait_ev((sem, val - 16))
        fn(self.eng.h).then_inc(sem, 16)
        self.n += 1
        ev = (sem, val)
        self.last[id(sem)] = ev
        for r in reads:
            r.r[id(sem)] = ev
        for w in writes:
            w.w = ev
            w.r = {}
        return ev


class T:
    def __init__(self, t, name):
        self.t = t
        self.res = Res(name)

    def __getitem__(self, k):
        return self.t[k]


def build_program(debug=None):
    nc = bass.Bass("TRN2", target_bir_lowering=False)
    dbg = {}

    def din(name, shape, dt=F32):
        return nc.dram_tensor(name, list(shape), dt, kind="ExternalInput").ap()

    x_d = din("x", [NSEQ, S, D])
    g_mix_d = din("g_mix", [D])
    w_in_d = din("w_in", [D, DIN])
    g_q_d = din("g_q", [128, 1])
    g_k_d = din("g_k", [128, 1])
    lam_d = din("lam4", [4, 64])
    g_subln_d = din("g_subln", [128, 1])
    strip_d = din("bias_strip", [NH, 128, STRIP_W])
    far_d = din("bias_far", [128, NH * 2])
    conv_w_d = din("conv_w", [128, 8, 4])
    conv_b_d = din("conv_b", [128, 8])
    gate_r_w_d = din("gate_r_w", [2, 8, 128, 128])
    gate_b_d = din("gate_b", [128, 2, 2, 8])
    gate_i_w_d = din("gate_i_w", [2, 8, 128, 128])
    lru_lambda_d = din("lru_lambda", [128, 2, 8])
    w_pa_d = din("w_proj_attn", [D, D])
    w_pl_d = din("w_proj_lru", [D, D])
    w_out_d = din("w_out", [D, D])
    g_ffn_d = din("g_ffn", [D])
    w_router_d = din("w_router", [128, 8, NE])
    w_gate_d = din("w_gate_e", [NE, D, DFF])
    w_up_d = din("w_up_e", [NE, D, DFF])
    w_down_d = din("w_down_e", [NE, DFF, D])
    out_d = nc.dram_tensor("out", [NSEQ, S, D], F32, kind="ExternalOutput").ap()
    hn_scr = nc.dram_tensor("hn_scr", [NSEQ * S, D], BF16).ap()

    def dout(name, shape, dt=F32):
        a = nc.dram_tensor(name, list(shape), dt, kind="ExternalOutput").ap()
        dbg[name] = a
        return a

    es = ExitStack()
    with es:
        def sem(name):
            return es.enter_context(nc.semaphore(name))

        PE = Eng("pe", nc.tensor, sem("s_pe"))
        ACT = Eng("act", nc.scalar, sem("s_act"))
        DVE = Eng("dve", nc.vector, sem("s_dve"))
        POOL = Eng("pool", nc.gpsimd, sem("s_pool"))
        SPE = Eng("sp", nc.sync, sem("s_sp"))
        ENGS = [PE, ACT, DVE, POOL, SPE]
        SPQ = DmaQ("spq", SPE, [sem(f"s_spq{i}") for i in range(16)])
        PLQ = DmaQ("plq", POOL, [sem(f"s_plq{i}") for i in range(16)])
        QS = [SPQ, PLQ]

        def barrier():
            evs = [(e.sem, e.count) for e in ENGS if e.count > 0]
            for q in QS:
                evs += list(q.last.values())
            for e in ENGS:
                for ev in evs:
                    e.wait_ev(ev)

        uid = [0]

        def sb(stack, name, shape, dt):
            uid[0] += 1
            name = f"{name}_u{uid[0]}"
            return T(stack.enter_context(nc.sbuf_tensor(name, list(shape), dt)), name)

        class TV:
            def __init__(self, ap, name):
                self.ap = ap
                self.res = Res(name)

            def __getitem__(self, k):
                return self.ap[k]

        spair = [es.enter_context(nc.psum_tensor(f"psp{i}", [128, 1024], F32)) for i in range(2)]
        banks = [TV(spair[i // 2][:, (i % 2) * 512:(i % 2 + 1) * 512], f"pb{i}") for i in range(4)]
        banks += [T(es.enter_context(nc.psum_tensor(f"pb{i}", [128, 512], F32)), f"pb{i}") for i in range(4, 8)]

        ident_bf = sb(es, "ident_bf", [128, 128], BF16)
        ident_f = sb(es, "ident_f", [128, 128], F32)
        ones_bf = sb(es, "ones_bf", [128, 128], BF16)
        ones_f = sb(es, "ones_f", [128, 128], F32)
        blk_bf = sb(es, "blk_bf", [128, 128], BF16)
        cst = Res("consts")
        gmix_b = sb(es, "gmix_b", [128, D], F32)
        gffn_b = sb(es, "gffn_b", [128, D], F32)
        gq2 = sb(es, "gq2", [128, 1], F32)
        gk2 = sb(es, "gk2", [128, 1], F32)
        gsub = sb(es, "gsub", [128, 1], F32)
        lam_t = sb(es, "lam_t", [128, 4 * 64], F32)
        lam_s = sb(es, "lam_s", [128, 4], F32)
        neg_lam = sb(es, "neg_lam", [128, 1], F32)
        far_t = sb(es, "far_t", [128, NH * 2], F32)
        convw = sb(es, "convw", [128, 8, 4], F32)
        convb = sb(es, "convb", [128, 8], F32)
        gbias = sb(es, "gbias", [128, 2, 2, 8], F32)
        nlogc = sb(es, "nlogc", [128, 2, 8], F32)
        nlogc2 = sb(es, "nlogc2", [128, 2, 8], F32)
        gw = sb(es, "gw", [128, 32, 128], BF16)
        wr_bf = sb(es, "wr_bf", [128, 8, NE], BF16)
        aff_all = sb(es, "aff_all", [128, 8, 4 * NE], F32)

        eps64 = sb(es, "eps64", [128, 1], F32)
        eps128 = sb(es, "eps128", [128, 1], F32)
        one_c = sb(es, "one_c", [128, 1], F32)

        def cmem(eng, ap, v):
            eng.op(lambda h: h.memset(ap, v), writes=[cst])

        cmem(DVE, ones_bf[:], 1.0)
        cmem(DVE, ones_f[:], 1.0)
        cmem(DVE, eps64[:], 64.0 * EPS)
        cmem(DVE, eps128[:], 128.0 * EPS)
        cmem(DVE, one_c[:], 1.0)
        cmem(DVE, blk_bf[:], 0.0)
        cmem(DVE, blk_bf[0:64, 0:64], 1.0)
        cmem(DVE, blk_bf[64:128, 64:128], 1.0)
        POOL.op(lambda h: h.affine_select(out=ident_f[:], in_=ones_f[:], pattern=[[1, 128]],
                                          compare_op=ALU.is_equal, fill=0.0, base=0, channel_multiplier=-1),
                reads=[cst], writes=[ident_f.res])
        DVE.op(lambda h: h.tensor_copy(out=ident_bf[:], in_=ident_f[:]), reads=[ident_f.res], writes=[ident_bf.res])

        def cld(q, out_ap, in_ap):
            q.dma(lambda h: h.dma_start(out=out_ap, in_=in_ap), writes=[cst])

        cld(SPQ, gmix_b[:], g_mix_d.partition_broadcast(128))
        cld(SPQ, gffn_b[:], g_ffn_d.partition_broadcast(128))
        cld(SPQ, gq2[:], g_q_d)
        cld(SPQ, gk2[:], g_k_d)
        cld(SPQ, gsub[:], g_subln_d)
        cld(SPQ, lam_t[:], lam_d.rearrange("a d -> (a d)").partition_broadcast(128))
        cld(SPQ, far_t[:], far_d)
        cld(SPQ, convw[:], conv_w_d)
        cld(SPQ, convb[:], conv_b_d)
        cld(SPQ, gbias[:], gate_b_d)
        cld(SPQ, nlogc[:], lru_lambda_d)
        cld(PLQ, gw[:, 0:16, :], gate_r_w_d.rearrange("a n c d -> c (a n) d"))
        cld(PLQ, gw[:, 16:32, :], gate_i_w_d.rearrange("a n c d -> c (a n) d"))
        cld(PLQ, wr_bf[:], w_router_d)

        lamp = lam_t[:].rearrange("p (a d) -> p a d", a=4)
        DVE.op(lambda h: h.tensor_tensor(out=lam_t[:, 0:64], in0=lam_t[:, 0:64], in1=lam_t[:, 64:128], op=ALU.mult),
               reads=[cst], writes=[cst])
        DVE.op(lambda h: h.tensor_tensor(out=lam_t[:, 128:192], in0=lam_t[:, 128:192], in1=lam_t[:, 192:256], op=ALU.mult),
               reads=[cst], writes=[cst])
        DVE.op(lambda h: h.reduce_sum(out=lam_s[:, 0:1], in_=lam_t[:, 0:64], axis=mybir.AxisListType.X),
               reads=[cst], writes=[cst])
        DVE.op(lambda h: h.reduce_sum(out=lam_s[:, 1:2], in_=lam_t[:, 128:192], axis=mybir.AxisListType.X),
               reads=[cst], writes=[cst])
        ACT.op(lambda h: h.activation(out=lam_s[:, 2:4], in_=lam_s[:, 0:2], func=AF.Exp), reads=[cst], writes=[cst])
        DVE.op(lambda h: h.scalar_tensor_tensor(out=neg_lam[:], in0=lam_s[:, 3:4], scalar=-LAM_INIT, in1=lam_s[:, 2:3],
                                                op0=ALU.add, op1=ALU.subtract), reads=[cst], writes=[cst])
        DVE.op(lambda h: h.tensor_scalar(out=gk2[:], in0=gk2[:], scalar1=8.0, scalar2=None, op0=ALU.mult),
               reads=[cst], writes=[cst])
        DVE.op(lambda h: h.tensor_scalar(out=gsub[:], in0=gsub[:], scalar1=math.sqrt(128.0) * (1.0 - LAM_INIT),
                                         scalar2=None, op0=ALU.mult), reads=[cst], writes=[cst])
        ACT.op(lambda h: h.activation(out=nlogc[:], in_=nlogc[:], func=AF.Exp, scale=-1.0), reads=[cst], writes=[cst])
        ACT.op(lambda h: h.activation(out=nlogc[:], in_=nlogc[:], func=AF.Ln, bias=one_c[:, 0:1]), reads=[cst], writes=[cst])
        DVE.op(lambda h: h.tensor_scalar(out=nlogc[:], in0=nlogc[:], scalar1=-8.0, scalar2=None, op0=ALU.mult),
               reads=[cst], writes=[cst])
        DVE.op(lambda h: h.tensor_scalar(out=nlogc2[:], in0=nlogc[:], scalar1=2.0, scalar2=None, op0=ALU.mult),
               reads=[cst], writes=[cst])
        barrier()

        w_in_r = w_in_d.rearrange("(c p) n -> p c n", p=128)

        def mm_group(bank_ap, pairs):
            n = len(pairs)
            return [(lambda h, i=i, l=l, r=r: h.matmul(bank_ap, l, r, start=(i == 0), stop=(i == n - 1)))
                    for i, (l, r) in enumerate(pairs)]

        seq_stack_outer = ExitStack()
        with seq_stack_outer as so:
            xnT = sb(so, "xnT", [128, 8, S], BF16)
            lruT = sb(so, "lruT", [128, 8, S], BF16)
            for s in range(NSEQ):
                with ExitStack() as p1:
                    NXB = 4
                    xb = [sb(p1, f"xb{i}", [128, D], F32) for i in range(NXB)]
                    xnb = [sb(p1, f"xnb{i}", [128, D], BF16) for i in range(NXB)]
                    ssq = [sb(p1, f"ssq{i}", [128, 1], F32) for i in range(NXB)]
                    for it in range(16 + 3):
                        t = it
                        if t < 16:
                            xt, xn, sq = xb[t % NXB], xnb[t % NXB], ssq[t % NXB]
                            SPQ.dma(lambda h, xt=xt, t=t: h.dma_start(out=xt[:], in_=x_d[s, t * 128:(t + 1) * 128, :]),
                                    writes=[xt.res])
                            ACT.op(lambda h, xt=xt, sq=sq, xn=xn: h.activation(out=xn[:], in_=xt[:], func=AF.Square, accum_out=sq[:]),
                                   reads=[xt.res], writes=[xn.res, sq.res])
                        t = it - 1
                        if 0 <= t < 16:
                            sq = ssq[t % NXB]
                            DVE.op(lambda h, sq=sq: h.tensor_scalar(out=sq[:], in0=sq[:], scalar1=1.0 / D, scalar2=EPS,
                                                                    op0=ALU.mult, op1=ALU.add), writes=[sq.res])
                            ACT.op(lambda h, sq=sq: h.activation(out=sq[:], in_=sq[:], func=AF.Ln), writes=[sq.res])
                            ACT.op(lambda h, sq=sq: h.activation(out=sq[:], in_=sq[:], func=AF.Exp, scale=-0.5), writes=[sq.res])
                        t = it - 2
                        if 0 <= t < 16:
                            xt, xn, sq = xb[t % NXB], xnb[t % NXB], ssq[t % NXB]
                            DVE.op(lambda h, xt=xt, xn=xn, sq=sq: h.scalar_tensor_tensor(
                                out=xn[:], in0=xt[:], scalar=sq[:, 0:1], in1=gmix_b[:], op0=ALU.mult, op1=ALU.mult),
                                reads=[xt.res, sq.res, cst], writes=[xn.res])
                        t = it - 3
                        if 0 <= t < 16:
                            xn = xnb[t % NXB]
                            bk = banks[t % 2]
                            bkb = bk[:].bitcast(BF16)
                            PE.op([(lambda h, dc=dc, xn=xn, bkb=bkb: h.transpose(
                                bkb[:, dc * 128:(dc + 1) * 128], xn[:, dc * 128:(dc + 1) * 128], ident_bf[:]))
                                for dc in range(8)], reads=[xn.res, ident_bf.res], writes=[bk.res])
                            if t % 2 == 0:
                                ACT.op(lambda h, bkb=bkb, t=t: h.copy(out=xnT[:, :, t * 128:(t + 1) * 128],
                                                                      in_=bkb.rearrange("p (c t) -> p c t", c=8)),
                                       reads=[bk.res], writes=[xnT.res])
                            else:
                                DVE.op(lambda h, bkb=bkb, t=t: h.tensor_copy(out=xnT[:, :, t * 128:(t + 1) * 128],
                                                                             in_=bkb.rearrange("p (c t) -> p c t", c=8)),
                                       reads=[bk.res], writes=[xnT.res])
                    barrier()
                if debug == "p1" and s == 0:
                    o = dout("dbg_xnT", [128, 8, S], BF16)
                    SPQ.dma(lambda h: h.dma_start(out=o, in_=xnT[:]), reads=[xnT.res])

                if debug in (None, "p2", "p4", "p5"):
                    with ExitStack() as p2:
                        wxb = [sb(p2, f"wx{i}", [128, 8, 128], BF16) for i in range(2)]
                        wyb = [sb(p2, f"wy{i}", [128, 8, 128], BF16) for i in range(2)]
                        xpad2 = [sb(p2, f"xpad{i}", [128, S + 4], F32) for i in range(2)]
                        ybuf = sb(p2, "ybuf", [128, S], F32)
                        xc2 = [sb(p2, f"xc{i}", [128, S], F32) for i in range(2)]
                        xcb2 = [sb(p2, f"xcb{i}", [128, S], BF16) for i in range(2)]
                        rbuf = [sb(p2, f"rbuf{i}", [128, S], F32) for i in range(2)]
                        ibuf = [sb(p2, f"ibuf{i}", [128, S], F32) for i in range(2)]
                        abuf = [sb(p2, f"abuf{i}", [128, S], F32) for i in range(2)]
                        tbuf = [sb(p2, f"tbuf{i}", [128, S], F32) for i in range(2)]
                        for xp_ in xpad2:
                            POOL.op(lambda h, xp_=xp_: h.memset(xp_[:, 0:2], 0.0), writes=[xp_.res])
                            POOL.op(lambda h, xp_=xp_: h.memset(xp_[:, S + 2:S + 4], 0.0), writes=[xp_.res])

                        def ld_lru_w(c):
                            wx, wy = wxb[c % 2], wyb[c % 2]
                            PLQ.dma(lambda h: h.dma_start(out=wx[:], in_=w_in_r[:, :, 3072 + c * 128:3072 + (c + 1) * 128]),
                                    writes=[wx.res])
                            PLQ.dma(lambda h: h.dma_start(out=wy[:], in_=w_in_r[:, :, 4096 + c * 128:4096 + (c + 1) * 128]),
                                    writes=[wy.res])

                        def proj_x(c):
                            wx, xpad = wxb[c % 2], xpad2[c % 2]
                            for tc in range(4):
                                bk = banks[tc]
                                PE.op(mm_group(bk[:], [(wx[:, dc, :], xnT[:, dc, tc * 512:(tc + 1) * 512]) for dc in range(8)]),
                                      reads=[wx.res, xnT.res], writes=[bk.res])
                                ACT.op(lambda h, bk=bk, tc=tc: h.copy(out=xpad[:, 2 + tc * 512:2 + (tc + 1) * 512], in_=bk[:]),
                                       reads=[bk.res], writes=[xpad.res])

                        def conv(c):
                            xpad, xc = xpad2[c % 2], xc2[c % 2]
                            DVE.op(lambda h: h.tensor_scalar(out=xc[:], in0=xpad[:, 0:S], scalar1=convw[:, c, 0:1],
                                                             scalar2=convb[:, c:c + 1], op0=ALU.mult, op1=ALU.add),
                                   reads=[xpad.res, cst], writes=[xc.res])
                            for k in range(1, 4):
                                DVE.op(lambda h, k=k: h.scalar_tensor_tensor(
                                    out=xc[:], in0=xpad[:, k:k + S], scalar=convw[:, c, k:k + 1], in1=xc[:],
                                    op0=ALU.mult, op1=ALU.add), reads=[xpad.res, cst], writes=[xc.res])

                        def cast_x(c):
                            xc, xcb = xc2[c % 2], xcb2[c % 2]
                            ACT.op(lambda h: h.copy(out=xcb[:], in_=xc[:]), reads=[xc.res], writes=[xcb.res])

                        def gates(c, dr):
                            xcb = xcb2[c % 2]
                            for gate, gb in ((0, rbuf[dr]), (1, ibuf[dr])):
                                for tc in range(4):
                                    bk = banks[4 + (gate * 2 + tc) % 4]
                                    wi = gate * 16 + dr * 8 + c
                                    PE.op(lambda h, bk=bk, wi=wi, tc=tc: h.matmul(
                                        bk[:], gw[:, wi, :], xcb[:, tc * 512:(tc + 1) * 512], start=True, stop=True),
                                        reads=[xcb.res, cst], writes=[bk.res])
                                    ACT.op(lambda h, bk=bk, gb=gb, tc=tc, gate=gate: h.activation(
                                        out=gb[:, tc * 512:(tc + 1) * 512], in_=bk[:], func=AF.Sigmoid,
                                        bias=gbias[:, gate, dr, c:c + 1]), reads=[bk.res, cst], writes=[gb.res])

                        def chain(c, dr):
                            rb, ib, ab, tb, xc = rbuf[dr], ibuf[dr], abuf[dr], tbuf[dr], xc2[c % 2]
                            DVE.op(lambda h: h.tensor_tensor(out=ib[:], in0=ib[:], in1=xc[:], op=ALU.mult),
                                   reads=[xc.res], writes=[ib.res])
                            ACT.op(lambda h: h.activation(out=ab[:], in_=rb[:], func=AF.Exp, scale=nlogc[:, dr, c:c + 1]),
                                   reads=[rb.res, cst], writes=[ab.res])
                            ACT.op(lambda h: h.activation(out=tb[:], in_=rb[:], func=AF.Exp, scale=nlogc2[:, dr, c:c + 1]),
                                   reads=[rb.res, cst], writes=[tb.res])
                            ACT.op(lambda h: h.activation(out=tb[:], in_=tb[:], func=AF.Ln, scale=-1.0, bias=one_c[:, 0:1]),
                                   reads=[cst], writes=[tb.res])
                            ACT.op(lambda h: h.activation(out=tb[:], in_=tb[:], func=AF.Exp, scale=0.5), writes=[tb.res])
                            DVE.op(lambda h: h.tensor_tensor(out=ib[:], in0=ib[:], in1=tb[:], op=ALU.mult),
                                   reads=[tb.res], writes=[ib.res])
                            if dr == 0:
                                DVE.op(lambda h: h.tensor_tensor_scan(out=tb[:], data0=ab[:], data1=ib[:], initial=0.0,
                                                                      op0=ALU.mult, op1=ALU.add),
                                       reads=[ab.res, ib.res], writes=[tb.res])
                            else:
                                DVE.op(lambda h: h.tensor_tensor_scan(out=tb[:, ::-1], data0=ab[:, ::-1], data1=ib[:, ::-1],
                                                                      initial=0.0, op0=ALU.mult, op1=ALU.add),
                                       reads=[ab.res, ib.res], writes=[tb.res])

                        ld_lru_w(0)
                        ld_lru_w(1)
                        proj_x(0)
                        conv(0)
                        cast_x(0)
                        for c in range(8):
                            wy = wyb[c % 2]
                            tg_ = abuf[1]
                            gates(c, 0)
                            chain(c, 0)
                            for tc in range(4):
                                bk = banks[tc]
                                PE.op(mm_group(bk[:], [(wy[:, dc, :], xnT[:, dc, tc * 512:(tc + 1) * 512]) for dc in range(8)]),
                                      reads=[wy.res, xnT.res], writes=[bk.res])
                                ACT.op(lambda h, bk=bk, tc=tc: h.copy(out=ybuf[:, tc * 512:(tc + 1) * 512], in_=bk[:]),
                                       reads=[bk.res], writes=[ybuf.res])
                            ACT.op(lambda h: h.activation(out=tg_[:], in_=ybuf[:], func=AF.Square),
                                   reads=[ybuf.res], writes=[tg_.res])
                            DVE.op(lambda h: h.tensor_scalar(out=tg_[:], in0=tg_[:], scalar1=0.044715, scalar2=1.0,
                                                             op0=ALU.mult, op1=ALU.add), writes=[tg_.res])
                            DVE.op(lambda h: h.tensor_tensor(out=tg_[:], in0=tg_[:], in1=ybuf[:], op=ALU.mult),
                                   reads=[ybuf.res], writes=[tg_.res])
                            if c + 1 < 8:
                                proj_x(c + 1)
                                conv(c + 1)
                            ACT.op(lambda h: h.activation(out=tg_[:], in_=tg_[:], func=AF.Sigmoid, scale=1.5957691216057308),
                                   writes=[tg_.res])
                            POOL.op(lambda h: h.tensor_tensor(out=ybuf[:], in0=tg_[:], in1=ybuf[:], op=ALU.mult),
                                    reads=[tg_.res], writes=[ybuf.res])
                            gates(c, 1)
                            chain(c, 1)
                            if c + 1 < 8:
                                cast_x(c + 1)
                            if c + 2 < 8:
                                ld_lru_w(c + 2)
                            POOL.op(lambda h: h.tensor_tensor(out=tbuf[0][:], in0=tbuf[0][:], in1=tbuf[1][:], op=ALU.add),
                                    reads=[tbuf[1].res], writes=[tbuf[0].res])
                            DVE.op(lambda h, c=c: h.tensor_tensor(out=lruT[:, c, :], in0=ybuf[:], in1=tbuf[0][:], op=ALU.mult),
                                   reads=[ybuf.res, tbuf[0].res], writes=[lruT.res])
                        barrier()
                    if debug == "p2" and s == 0:
                        o = dout("dbg_lruT", [128, 8, S], BF16)
                        SPQ.dma(lambda h: h.dma_start(out=o, in_=lruT[:]), reads=[lruT.res])

                p34 = ExitStack()
                oT = sb(p34, "oT", [128, 8, S], BF16)
                if debug in (None, "p3", "p4", "p5"):
                    with ExitStack() as p3:
                        wq = [sb(p3, f"wq{i}", [128, 8, 128], BF16) for i in range(2)]
                        wk = [sb(p3, f"wk{i}", [128, 8, 128], BF16) for i in range(2)]
                        wv = [sb(p3, f"wv{i}", [128, 8, 128], BF16) for i in range(2)]
                        qTh2 = [sb(p3, f"qTh{i}", [128, S], BF16) for i in range(2)]
                        kTz2 = [[sb(p3, f"kTz{i}_{m}", [128, S], BF16) for m in range(2)] for i in range(2)]
                        for i_ in range(2):
                            for m_ in range(2):
                                POOL.op(lambda h, i_=i_, m_=m_: h.memset(kTz2[i_][m_][:], 0.0), writes=[kTz2[i_][m_].res])
                        Vh2 = [sb(p3, f"Vh{i}", [128, 16, 128], BF16) for i in range(2)]
                        sqb = [sb(p3, f"sqb{i}", [128, 512], BF16) for i in range(2)]
                        rsb = [sb(p3, f"rsb{i}", [128, 512], F32) for i in range(2)]
                        NEB = 4
                        Eb = [sb(p3, f"E_{i}", [128, 1024], BF16) for i in range(NEB)]
                        rz = [sb(p3, f"rz{i}", [128, 512], F32) for i in range(2)]
                        t1 = sb(p3, "t1", [128, 512], F32)
                        t2 = sb(p3, "t2", [128, 512], F32)
                        osq = sb(p3, "osq", [128, 512], BF16)
                        ors = rz[0]
                        zacc = [sb(p3, f"zacc{i}", [128, 512], F32) for i in range(2)]
                        zbf = sb(p3, "zbf", [128, 1024], BF16)
                        bO = [banks[4], banks[5]]
                        bA, bB = banks[6], banks[7]
                        strip_f = sb(p3, "strip_f", [128, STRIP_W], F32)
                        strip_hi = [sb(p3, f"strip_hi{i}", [128, STRIP_W], BF16) for i in range(2)]
                        strip_lo = [sb(p3, f"strip_lo{i}", [128, STRIP_W], BF16) for i in range(2)]

                        def ld_attn_w(hd_):
                            SPQ.dma(lambda h: h.dma_start(out=strip_f[:], in_=strip_d[hd_]), writes=[strip_f.res])
                            shi, slo = strip_hi[hd_ % 2], strip_lo[hd_ % 2]
                            DVE.op(lambda h: h.tensor_copy(out=shi[:], in_=strip_f[:]), reads=[strip_f.res], writes=[shi.res])
                            DVE.op(lambda h: h.tensor_tensor(out=slo[:], in0=strip_f[:], in1=shi[:], op=ALU.subtract),
                                   reads=[strip_f.res, shi.res], writes=[slo.res])
                            for (wt, off) in ((wq[hd_ % 2], 0), (wk[hd_ % 2], 1024), (wv[hd_ % 2], 2048)):
                                PLQ.dma(lambda h, wt=wt, off=off: h.dma_start(
                                    out=wt[:], in_=w_in_r[:, :, off + hd_ * 128:off + (hd_ + 1) * 128]), writes=[wt.res])

                        bB_lock = [None]
                        DONE = object()

                        def acquire(me):
                            while bB_lock[0] not in (None, me):
                                yield
                            bB_lock[0] = me

                        class BG:
                            fin = []
                            gen = None

                            @staticmethod
                            def step():
                                if BG.fin:
                                    if next(BG.fin[0], DONE) is DONE:
                                        BG.fin.pop(0)
                                if BG.gen is not None:
                                    if next(BG.gen, DONE) is DONE:
                                        BG.gen = None

                            @staticmethod
                            def drain_gen():
                                while BG.gen is not None:
                                    BG.step()

                            @staticmethod
                            def drain_all():
                                while BG.gen is not None or BG.fin:
                                    BG.step()

                        BG.fin = []
                        BG.gen = None

                        def proj_steps(hd_):
                            qT_, kT_, V_ = qTh2[hd_ % 2], kTz2[hd_ % 2], Vh2[hd_ % 2]
                            i2 = 0
                            for (wt, dst, gsc) in ((wq[hd_ % 2], qT_, gq2), (wk[hd_ % 2], kT_, gk2)):
                                for tc in range(4):
                                    sq_, rs_ = sqb[i2 % 2], rsb[i2 % 2]
                                    i2 += 1
                                    PE.op(mm_group(bA[:], [(wt[:, dc, :], xnT[:, dc, tc * 512:(tc + 1) * 512]) for dc in range(8)]),
                                          reads=[wt.res, xnT.res], writes=[bA.res])
                                    yield
                                    ACT.op(lambda h, sq_=sq_: h.activation(out=sq_[:], in_=bA[:], func=AF.Square),
                                           reads=[bA.res], writes=[sq_.res])
                                    yield
                                    yield from acquire("p")
                                    PE.op(lambda h, sq_=sq_: h.matmul(bB[:], blk_bf[:], sq_[:], start=True, stop=True),
                                          reads=[sq_.res, cst], writes=[bB.res])
                                    yield
                                    ACT.op(lambda h, rs_=rs_: h.activation(out=rs_[:], in_=bB[:], func=AF.Ln, bias=eps64[:, 0:1]),
                                           reads=[bB.res, cst], writes=[rs_.res])
                                    ACT.op(lambda h, rs_=rs_: h.activation(out=rs_[:], in_=rs_[:], func=AF.Exp, scale=-0.5),
                                           writes=[rs_.res])
                                    bB_lock[0] = None
                                    yield
                                    if isinstance(dst, list):
                                        for m_ in range(2):
                                            ps_ = slice(m_ * 64, (m_ + 1) * 64)
                                            DVE.op(lambda h, rs_=rs_, dst=dst, gsc=gsc, tc=tc, m_=m_, ps_=ps_: h.scalar_tensor_tensor(
                                                out=dst[m_][ps_, tc * 512:(tc + 1) * 512], in0=bA[ps_, :], scalar=gsc[ps_, 0:1],
                                                in1=rs_[ps_, :], op0=ALU.mult, op1=ALU.mult),
                                                reads=[bA.res, rs_.res, cst], writes=[dst[m_].res])
                                    else:
                                        DVE.op(lambda h, rs_=rs_, dst=dst, gsc=gsc, tc=tc: h.scalar_tensor_tensor(
                                            out=dst[:, tc * 512:(tc + 1) * 512], in0=bA[:], scalar=gsc[:, 0:1], in1=rs_[:],
                                            op0=ALU.mult, op1=ALU.mult), reads=[bA.res, rs_.res, cst], writes=[dst.res])
                                    yield
                            wv_ = wv[hd_ % 2]
                            for tg in range(4):
                                fl = []
                                for j in range(4):
                                    tt = tg * 4 + j
                                    fl += mm_group(bA[:, j * 128:(j + 1) * 128],
                                                   [(xnT[:, dc, tt * 128:(tt + 1) * 128], wv_[:, dc, :]) for dc in range(8)])
                                PE.op(fl, reads=[wv_.res, xnT.res], writes=[bA.res])
                                yield
                                ACT.op(lambda h, tg=tg: h.copy(out=V_[:, tg * 4:(tg + 1) * 4, :],
                                                               in_=bA[:].rearrange("p (j v) -> p j v", j=4)),
                                       reads=[bA.res], writes=[V_.res])
                                yield

                        def fin_steps(hd_, qs):
                            for m in range(2):
                                yield from acquire("f")
                                PE.op(lambda h, m=m: h.matmul(bB[:], ones_bf[:], zbf[:, m * 512:(m + 1) * 512], start=True, stop=True),
                                      reads=[zbf.res, cst], writes=[bB.res])
                                yield
                                ACT.op(lambda h, m=m: h.activation(out=rz[m][:], in_=bB[:], func=AF.Ln),
                                       reads=[bB.res], writes=[rz[m].res])
                                ACT.op(lambda h, m=m: h.activation(out=rz[m][:], in_=rz[m][:], func=AF.Exp, scale=-1.0),
                                       writes=[rz[m].res])
                                bB_lock[0] = None
                                yield
                            DVE.op(lambda h: h.tensor_tensor(out=t1[:], in0=t1[:], in1=rz[0][:], op=ALU.mult),
                                   reads=[rz[0].res], writes=[t1.res])
                            DVE.op(lambda h: h.tensor_tensor(out=t2[:], in0=t2[:], in1=rz[1][:], op=ALU.mult),
                                   reads=[rz[1].res], writes=[t2.res])
                            DVE.op(lambda h: h.scalar_tensor_tensor(out=t1[:], in0=t2[:], scalar=neg_lam[:, 0:1], in1=t1[:],
                                                                    op0=ALU.mult, op1=ALU.add),
                                   reads=[t2.res, cst], writes=[t1.res])
                            yield
                            ACT.op(lambda h: h.activation(out=osq[:], in_=t1[:], func=AF.Square),
                                   reads=[t1.res], writes=[osq.res])
                            yield
                            yield from acquire("f")
                            PE.op(lambda h: h.matmul(bB[:], ones_bf[:], osq[:], start=True, stop=True),
                                  reads=[osq.res, cst], writes=[bB.res])
                            yield
                            ACT.op(lambda h: h.activation(out=ors[:], in_=bB[:], func=AF.Ln, bias=eps128[:, 0:1]),
                                   reads=[bB.res, cst], writes=[ors.res])
                            ACT.op(lambda h: h.activation(out=ors[:], in_=ors[:], func=AF.Exp, scale=-0.5), writes=[ors.res])
                            bB_lock[0] = None
                            yield
                            DVE.op(lambda h: h.scalar_tensor_tensor(out=oT[:, hd_, qs], in0=t1[:], scalar=gsub[:, 0:1], in1=ors[:],
                                                                    op0=ALU.mult, op1=ALU.mult),
                                   reads=[t1.res, ors.res, cst], writes=[oT.res])
                            yield

                        ld_attn_w(0)
                        BG.gen = proj_steps(0)
                        BG.drain_gen()
                        ecnt = 0
                        for hd_ in range(NH):
                            if hd_ + 1 < NH:
                                ld_attn_w(hd_ + 1)
                                BG.gen = proj_steps(hd_ + 1)
                            qTh, kTz, Vh = qTh2[hd_ % 2], kTz2[hd_ % 2], Vh2[hd_ % 2]
                            for qc in range(4):
                                qs = slice(qc * 512, (qc + 1) * 512)

                                def is_far(kc):
                                    delta = kc * 128 - qc * 512
                                    return delta >= 602 or delta <= -218

                                def emit_S(kc):
                                    p_ = kc % 2
                                    delta = kc * 128 - qc * 512
                                    for m in range(2):
                                        bk_ = banks[p_ * 2 + m]
                                        if is_far(kc):
                                            PE.op(lambda h, m=m, kc=kc, bk_=bk_: h.matmul(
                                                bk_[:], kTz[m][:, kc * 128:(kc + 1) * 128], qTh[:, qs], start=True, stop=True),
                                                reads=[kTz[m].res, qTh.res], writes=[bk_.res])
                                        else:
                                            u0 = 512 - delta
                                            shi, slo = strip_hi[hd_ % 2], strip_lo[hd_ % 2]
                                            PE.op([lambda h, m=m, kc=kc, bk_=bk_: h.matmul(
                                                       bk_[:], kTz[m][:, kc * 128:(kc + 1) * 128], qTh[:, qs], start=True, stop=False),
                                                   lambda h, bk_=bk_, u0=u0, shi=shi: h.matmul(
                                                       bk_[:], ident_bf[:], shi[:, u0:u0 + 512], start=False, stop=False),
                                                   lambda h, bk_=bk_, u0=u0, slo=slo: h.matmul(
                                                       bk_[:], ident_bf[:], slo[:, u0:u0 + 512], start=False, stop=True)],
                                                  reads=[kTz[m].res, qTh.res, shi.res, slo.res, ident_bf.res], writes=[bk_.res])

                                def emit_exp(kc):
                                    nonlocal ecnt
                                    delta = kc * 128 - qc * 512
                                    p_ = kc % 2
                                    b0_, b1_ = banks[p_ * 2], banks[p_ * 2 + 1]
                                    E_ = Eb[ecnt % NEB]
                                    ecnt += 1
                                    if is_far(kc):
                                        col = hd_ * 2 + (0 if delta > 0 else 1)
                                        ACT.op(lambda h, E_=E_, col=col, p_=p_: h.activation(
                                            out=E_[:], in_=spair[p_][:], func=AF.Exp, bias=far_t[:, col:col + 1]),
                                            reads=[b0_.res, b1_.res, cst], writes=[E_.res])
                                    else:
                                        ACT.op(lambda h, E_=E_, p_=p_: h.activation(out=E_[:], in_=spair[p_][:], func=AF.Exp),
                                               reads=[b0_.res, b1_.res], writes=[E_.res])
                                    return E_

                                def emit_PV(kc, E_):
                                    PE.op([lambda h, kc=kc: h.matmul(bO[0][:], Vh[:, kc, :], E_[:, 0:512],
                                                                     start=(kc == 0), stop=(kc == 15)),
                                           lambda h, kc=kc: h.matmul(bO[1][:], Vh[:, kc, :], E_[:, 512:1024],
                                                                     start=(kc == 0), stop=(kc == 15))],
                                          reads=[Vh.res, E_.res], writes=[bO[0].res, bO[1].res])
                                    for m, eng in ((0, POOL), (1, DVE)):
                                        if kc == 0:
                                            eng.op(lambda h, m=m: h.tensor_copy(out=zacc[m][:], in_=E_[:, m * 512:(m + 1) * 512]),
                                                   reads=[E_.res], writes=[zacc[m].res])
                                        else:
                                            eng.op(lambda h, m=m: h.tensor_tensor(out=zacc[m][:], in0=zacc[m][:],
                                                                                  in1=E_[:, m * 512:(m + 1) * 512], op=ALU.add),
                                                   reads=[E_.res], writes=[zacc[m].res])

                                emit_S(0)
                                emit_S(1)
                                prev = None
                                for kc in range(16):
                                    Es = emit_exp(kc)
                                    if prev is not None:
                                        emit_PV(kc - 1, prev)
                                    if kc + 2 < 16:
                                        emit_S(kc + 2)
                                    prev = Es
                                    BG.step()
                                emit_PV(15, prev)
                                ACT.op(lambda h: h.copy(out=t1[:], in_=bO[0][:]), reads=[bO[0].res], writes=[t1.res])
                                DVE.op(lambda h: h.tensor_copy(out=t2[:], in_=bO[1][:]), reads=[bO[1].res], writes=[t2.res])
                                for m in range(2):
                                    DVE.op(lambda h, m=m: h.tensor_copy(out=zbf[:, m * 512:(m + 1) * 512], in_=zacc[m][:]),
                                           reads=[zacc[m].res], writes=[zbf.res])
                                BG.fin.append(fin_steps(hd_, qs))
                            BG.drain_gen()
                        BG.drain_all()
                        barrier()
                    if debug == "p3" and s == 0:
                        o = dout("dbg_oT", [128, 8, S], BF16)
                        SPQ.dma(lambda h: h.dma_start(out=o, in_=oT[:]), reads=[oT.res])

                if debug in (None, "p4", "p5"):
                    with ExitStack() as p4:
                        NTH = 2
                        TH = S // NTH
                        wpc = [[sb(p4, f"wpc{k}_{i}", [128, 8, 128], BF16) for i in range(2)] for k in range(4)]
                        wo = sb(p4, "wo", [128, 8, D], BF16)
                        w_pa_r = w_pa_d.rearrange("(c p) n -> p c n", p=128)
                        w_pl_r = w_pl_d.rearrange("(c p) n -> p c n", p=128)
                        w_out_r = w_out_d.rearrange("(c p) n -> p c n", p=128)
                        for half in range(2):
                            PLQ.dma(lambda h, half=half: h.dma_start(
                                out=wo[:, half * 4:(half + 1) * 4, :], in_=w_out_r[:, half * 4:(half + 1) * 4, :]),
                                writes=[wo.res])
                        sg = [sb(p4, f"sg{i}", [128, 512], BF16) for i in range(4)]
                        mixA = [sb(p4, f"mixA{i}", [128, 512], F32) for i in range(2)]
                        mixT = sb(p4, "mixT", [128, 8, TH], BF16)
                        hbuf = [sb(p4, f"hbuf{i}", [128, D], F32) for i in range(4)]
                        hnb = [sb(p4, f"hnb{i}", [128, D], BF16) for i in range(3)]
                        ssq4 = [sb(p4, f"ssq4{i}", [128, 1], F32) for i in range(4)]
                        hnT = [sb(p4, f"hnT{i}", [128, 8, 128], BF16) for i in range(2)]
                        lg = [sb(p4, f"lg{i}", [128, NE], F32) for i in range(2)]
                        lsum = [sb(p4, f"lsum{i}", [128, 1], F32) for i in range(2)]
                        hres = [Res(f"out_rows{s}")]

                        def ld_p4_w(i):
                            db = i % 8
                            dsl = slice(db * 128, (db + 1) * 128)
                            srcs = (w_in_r[:, :, 5120 + db * 128:5120 + (db + 1) * 128],
                                    w_in_r[:, :, 6144 + db * 128:6144 + (db + 1) * 128],
                                    w_pa_r[:, :, dsl], w_pl_r[:, :, dsl])
                            for k in range(4):
                                wt = wpc[k][i % 2]
                                PLQ.dma(lambda h, wt=wt, k=k: h.dma_start(out=wt[:], in_=srcs[k]), writes=[wt.res])

                        ld_p4_w(0)
                        sgc = 0
                        itc = 0
                        for th in range(NTH):
                            for db in range(8):
                                i4 = th * 8 + db
                                if i4 + 1 < NTH * 8:
                                    ld_p4_w(i4 + 1)
                                wga_, wgr_, wpa_, wpl_ = (wpc[k][i4 % 2] for k in range(4))
                                for tcl in range(TH // 512):
                                    tc = th * (TH // 512) + tcl
                                    ts_ = slice(tc * 512, (tc + 1) * 512)
                                    sga, sgr = sg[sgc % 4], sg[(sgc + 1) % 4]
                                    sgc += 2
                                    mA = mixA[itc % 2]
                                    bo = (itc % 2) * 4
                                    itc += 1
                                    b0, b1, b2, b3 = banks[bo], banks[bo + 1], banks[bo + 2], banks[bo + 3]
                                    PE.op(mm_group(b0[:], [(wga_[:, dc, :], xnT[:, dc, ts_]) for dc in range(8)]),
                                          reads=[wga_.res, xnT.res], writes=[b0.res])
                                    ACT.op(lambda h, sga=sga, b0=b0: h.activation(out=sga[:], in_=b0[:], func=AF.Sigmoid),
                                           reads=[b0.res], writes=[sga.res])
                                    PE.op(mm_group(b1[:], [(wgr_[:, dc, :], xnT[:, dc, ts_]) for dc in range(8)]),
                                          reads=[wgr_.res, xnT.res], writes=[b1.res])
                                    ACT.op(lambda h, sgr=sgr, b1=b1: h.activation(out=sgr[:], in_=b1[:], func=AF.Sigmoid),
                                           reads=[b1.res], writes=[sgr.res])
                                    PE.op(mm_group(b2[:], [(wpa_[:, ac, :], oT[:, ac, ts_]) for ac in range(8)]),
                                          reads=[wpa_.res, oT.res], writes=[b2.res])
                                    DVE.op(lambda h, mA=mA, sga=sga, b2=b2: h.tensor_tensor(out=mA[:], in0=b2[:], in1=sga[:], op=ALU.mult),
                                           reads=[b2.res, sga.res], writes=[mA.res])
                                    PE.op(mm_group(b3[:], [(wpl_[:, ac, :], lruT[:, ac, ts_]) for ac in range(8)]),
                                          reads=[wpl_.res, lruT.res], writes=[b3.res])
                                    DVE.op(lambda h, sgr=sgr, b3=b3: h.tensor_tensor(out=sgr[:], in0=b3[:], in1=sgr[:], op=ALU.mult),
                                           reads=[b3.res], writes=[sgr.res])
                                    POOL.op(lambda h, mA=mA, sgr=sgr, db=db, tcl=tcl: h.tensor_tensor(
                                        out=mixT[:, db, tcl * 512:(tcl + 1) * 512], in0=mA[:], in1=sgr[:], op=ALU.add),
                                        reads=[mA.res, sgr.res], writes=[mixT.res])
                            NTT = TH // 128
                            for it4 in range(NTT + 4):
                                ttl = it4
                                if ttl < NTT:
                                    gt = th * NTT + ttl
                                    rows = slice(gt * 128, (gt + 1) * 128)
                                    hb = hbuf[gt % 4]
                                    SPQ.dma(lambda h, hb=hb, rows=rows: h.dma_start(out=hb[:], in_=x_d[s, rows, :]), writes=[hb.res])
                                    for eh in range(2):
                                        bk = banks[(gt % 2) * 4 + eh]
                                        PE.op(mm_group(bk[:], [(mixT[:, dc, ttl * 128:(ttl + 1) * 128], wo[:, dc, eh * 512:(eh + 1) * 512])
                                                               for dc in range(8)]), reads=[mixT.res, wo.res], writes=[bk.res])
                                        DVE.op(lambda h, bk=bk, hb=hb, eh=eh: h.tensor_tensor(
                                            out=hb[:, eh * 512:(eh + 1) * 512], in0=bk[:], in1=hb[:, eh * 512:(eh + 1) * 512], op=ALU.add),
                                            reads=[bk.res], writes=[hb.res])
                                ttl = it4 - 1
                                if 0 <= ttl < NTT:
                                    gt = th * NTT + ttl
                                    rows = slice(gt * 128, (gt + 1) * 128)
                                    hb, hn, sq4 = hbuf[gt % 4], hnb[gt % 3], ssq4[gt % 4]
                                    SPQ.dma(lambda h, hb=hb, rows=rows: h.dma_start(out=out_d[s, rows, :], in_=hb[:]),
                                            reads=[hb.res], writes=[hres[0]])
                                    ACT.op(lambda h, hb=hb, sq4=sq4, hn=hn: h.activation(out=hn[:], in_=hb[:], func=AF.Square, accum_out=sq4[:]),
                                           reads=[hb.res], writes=[hn.res, sq4.res])
                                ttl = it4 - 2
                                if 0 <= ttl < NTT:
                                    gt = th * NTT + ttl
                                    hb, hn, sq4 = hbuf[gt % 4], hnb[gt % 3], ssq4[gt % 4]
                                    DVE.op(lambda h, sq4=sq4: h.tensor_scalar(out=sq4[:], in0=sq4[:], scalar1=1.0 / D, scalar2=EPS,
                                                                              op0=ALU.mult, op1=ALU.add), writes=[sq4.res])
                                    ACT.op(lambda h, sq4=sq4: h.activation(out=sq4[:], in_=sq4[:], func=AF.Ln), writes=[sq4.res])
                                    ACT.op(lambda h, sq4=sq4: h.activation(out=sq4[:], in_=sq4[:], func=AF.Exp, scale=-0.5), writes=[sq4.res])
                                    DVE.op(lambda h, hb=hb, hn=hn, sq4=sq4: h.scalar_tensor_tensor(
                                        out=hn[:], in0=hb[:], scalar=sq4[:, 0:1], in1=gffn_b[:], op0=ALU.mult, op1=ALU.mult),
                                        reads=[hb.res, sq4.res, cst], writes=[hn.res])
                                    SPQ.dma(lambda h, hn=hn, gt=gt: h.dma_start(
                                        out=hn_scr[s * S + gt * 128:s * S + (gt + 1) * 128, :], in_=hn[:]),
                                        reads=[hn.res], writes=[hres[0]])
                                ttl = it4 - 3
                                if 0 <= ttl < NTT:
                                    gt = th * NTT + ttl
                                    hn, hT = hnb[gt % 3], hnT[gt % 2]
                                    bk = banks[2 + (gt % 2) * 4]
                                    bkb = bk[:].bitcast(BF16)
                                    PE.op([(lambda h, dc=dc, hn=hn, bkb=bkb: h.transpose(
                                        bkb[:, dc * 128:(dc + 1) * 128], hn[:, dc * 128:(dc + 1) * 128], ident_bf[:]))
                                        for dc in range(8)], reads=[hn.res, ident_bf.res], writes=[bk.res])
                                    ACT.op(lambda h, hT=hT, bkb=bkb: h.copy(out=hT[:], in_=bkb.rearrange("p (c t) -> p c t", c=8)),
                                           reads=[bk.res], writes=[hT.res])
                                ttl = it4 - 4
                                if 0 <= ttl < NTT:
                                    gt = th * NTT + ttl
                                    hT, lg_, ls_ = hnT[gt % 2], lg[gt % 2], lsum[gt % 2]
                                    bk7 = banks[3 + (gt % 2) * 4]
                                    PE.op(mm_group(bk7[:, 0:NE], [(hT[:, dc, :], wr_bf[:, dc, :]) for dc in range(8)]),
                                          reads=[hT.res, cst], writes=[bk7.res])
                                    ACT.op(lambda h, lg_=lg_, ls_=ls_, bk7=bk7: h.activation(out=lg_[:], in_=bk7[:, 0:NE], func=AF.Exp,
                                                                                             accum_out=ls_[:]),
                                           reads=[bk7.res], writes=[lg_.res, ls_.res])
                                    DVE.op(lambda h, ls_=ls_: h.reciprocal(out=ls_[:], in_=ls_[:]), writes=[ls_.res])
                                    DVE.op(lambda h, lg_=lg_, ls_=ls_, gt=gt: h.tensor_scalar(
                                        out=aff_all[:, gt % 8, (gt // 8) * 2 * NE + s * NE:(gt // 8) * 2 * NE + (s + 1) * NE], in0=lg_[:], scalar1=ls_[:, 0:1], scalar2=None,
                                        op0=ALU.mult), reads=[lg_.res, ls_.res], writes=[cst])
                        barrier()
                    if debug == "p4" and s == 0:
                        barrier()
                p34.close()
            barrier()

        if debug in (None, "p5"):
            with ExitStack() as p5:
                HS = S // 2
                affT = sb(p5, "affT", [64, HS], F32)
                work = sb(p5, "work", [64, HS], F32)
                hvals = sb(p5, "hvals", [64, CAP], F32)
                idxu = sb(p5, "idxu", [64, CAP], U32)
                hidx = sb(p5, "hidx", [64, CAP], F32)
                bshv = sb(p5, "bshv", [32, CAP], F32)
                bshi = sb(p5, "bshi", [32, CAP], F32)
                msk = sb(p5, "msk", [32, CAP], U32)
                vals = sb(p5, "vals", [32, CAP], F32)
                idxf = sb(p5, "idxf", [32, CAP], F32)
                idxT = sb(p5, "idxT", [128, 2, 32], I32)
                idxTf = sb(p5, "idxTf", [128, 2, 32], F32)
                valT = sb(p5, "valT", [128, 2, 32], F32)
                wgb = [sb(p5, f"wgb{i}", [128, 8, 512], BF16) for i in range(3)]
                wub = [sb(p5, f"wub{i}", [128, 8, 512], BF16) for i in range(3)]
                wdb = [sb(p5, f"wdb{i}", [128, 16, 512], BF16) for i in range(2)]
                xg = [sb(p5, f"xg{i}", [128, D], BF16) for i in range(4)]
                xgT = [sb(p5, f"xgT{i}", [128, 8, 512], BF16) for i in range(2)]
                hgT = [sb(p5, f"hgT{i}", [128, 16, 512], BF16) for i in range(2)]
                silu = [sb(p5, f"silu{i}", [128, 512], F32) for i in range(2)]
                ye = [sb(p5, f"ye{i}", [128, D], F32) for i in range(4)]
                w_gate_r = w_gate_d.rearrange("e (c p) f -> e p c f", p=128)
                w_up_r = w_up_d.rearrange("e (c p) f -> e p c f", p=128)
                w_down_r = w_down_d.rearrange("e (c p) d -> e p c d", p=128)
                out_flat = out_d.rearrange("s t d -> (s t) d")

                def ld_gu(e, fg, slot):
                    PLQ.dma(lambda h: h.dma_start(out=wgb[slot][:], in_=w_gate_r[e, :, :, fg * 512:(fg + 1) * 512]),
                            writes=[wgb[slot].res])
                    PLQ.dma(lambda h: h.dma_start(out=wub[slot][:], in_=w_up_r[e, :, :, fg * 512:(fg + 1) * 512]),
                            writes=[wub[slot].res])

                def ld_down(e, dh):
                    wd = wdb[dh]
                    for q2 in range(2):
                        PLQ.dma(lambda h, q2=q2: h.dma_start(out=wd[:, q2 * 8:(q2 + 1) * 8, :],
                                                             in_=w_down_r[e, :, q2 * 8:(q2 + 1) * 8, dh * 512:(dh + 1) * 512]),
                                writes=[wd.res])

                def gather(e):
                    for st in range(4):
                        sq_, hf = st // 2, st % 2
                        row = sq_ * NE + e
                        xg_ = xg[st]
                        PLQ.dma(lambda h, xg_=xg_, hf=hf, row=row: h.indirect_dma_start(
                            out=xg_[:], out_offset=None, in_=hn_scr,
                            in_offset=bass.IndirectOffsetOnAxis(ap=idxT[:, hf, row:row + 1], axis=0)),
                            reads=[idxT.res], writes=[xg_.res])

                for j_ in range(3):
                    ld_gu(0, j_, j_)
                ld_down(0, 0)
                ld_down(0, 1)
                for g in range(2):
                    bk = banks[g]
                    PE.op([(lambda h, j=j, bk=bk, g=g: h.transpose(bk[0:64, j * 128:(j + 1) * 128], aff_all[:, g * 4 + j, :], ident_f[:]))
                           for j in range(4)], reads=[cst, ident_f.res], writes=[bk.res])
                    DVE.op(lambda h, bk=bk, g=g: h.tensor_copy(out=affT[:, g * 512:(g + 1) * 512], in_=bk[0:64, :]),
                           reads=[bk.res], writes=[affT.res])
                for r in range(CAP // 8):
                    src = affT if r == 0 else work
                    DVE.op(lambda h, r=r, src=src: h.max(out=hvals[:, r * 8:(r + 1) * 8], in_=src[:]),
                           reads=[src.res], writes=[hvals.res])
                    DVE.op(lambda h, r=r, src=src: h.max_index(out=idxu[:, r * 8:(r + 1) * 8], in_max=hvals[:, r * 8:(r + 1) * 8],
                                                             in_values=src[:]), reads=[src.res, hvals.res], writes=[idxu.res])
                    if r + 1 < CAP // 8:
                        DVE.op(lambda h, r=r, src=src: h.match_replace(out=work[:], in_to_replace=hvals[:, r * 8:(r + 1) * 8],
                                                                     in_values=src[:], imm_value=NEG_BIG),
                               reads=[src.res, hvals.res], writes=[work.res])
                DVE.op(lambda h: h.tensor_copy(out=hidx[:], in_=idxu[:]), reads=[idxu.res], writes=[hidx.res])
                DVE.op(lambda h: h.tensor_scalar(out=hidx[32:64, :], in0=hidx[32:64, :], scalar1=float(HS), scalar2=None, op0=ALU.add),
                       writes=[hidx.res])
                SPQ.dma(lambda h: h.dma_start(out=bshv[:], in_=hvals[32:64, :]), reads=[hvals.res], writes=[bshv.res])
                SPQ.dma(lambda h: h.dma_start(out=bshi[:], in_=hidx[32:64, :]), reads=[hidx.res], writes=[bshi.res])
                DVE.op(lambda h: h.tensor_tensor(out=msk[:], in0=hvals[0:32, :], in1=bshv[:, ::-1], op=ALU.is_gt),
                       reads=[hvals.res, bshv.res], writes=[msk.res])
                DVE.op(lambda h: h.tensor_tensor(out=vals[:], in0=hvals[0:32, :], in1=bshv[:, ::-1], op=ALU.max),
                       reads=[hvals.res, bshv.res], writes=[vals.res])
                DVE.op(lambda h: h.tensor_copy(out=idxf[:], in_=bshi[:, ::-1]), reads=[bshi.res], writes=[idxf.res])
                DVE.op(lambda h: h.copy_predicated(out=idxf[:], mask=msk[:], data=hidx[0:32, :]),
                       reads=[msk.res, hidx.res], writes=[idxf.res])
                for hf in range(2):
                    bk = banks[2]
                    PE.op(lambda h, hf=hf, bk=bk: h.transpose(bk[:, 0:32], idxf[:, hf * 128:(hf + 1) * 128], ident_f[0:32, 0:32]),
                          reads=[idxf.res, ident_f.res], writes=[bk.res])
                    DVE.op(lambda h, hf=hf, bk=bk: h.tensor_copy(out=idxTf[:, hf, :], in_=bk[:, 0:32]),
                           reads=[bk.res], writes=[idxTf.res])
                    bk = banks[3]
                    PE.op(lambda h, hf=hf, bk=bk: h.transpose(bk[:, 0:32], vals[:, hf * 128:(hf + 1) * 128], ident_f[0:32, 0:32]),
                          reads=[vals.res, ident_f.res], writes=[bk.res])
                    DVE.op(lambda h, hf=hf, bk=bk: h.tensor_copy(out=valT[:, hf, :], in_=bk[:, 0:32]),
                           reads=[bk.res], writes=[valT.res])
                DVE.op(lambda h: h.tensor_scalar(out=idxTf[:, :, NE:2 * NE], in0=idxTf[:, :, NE:2 * NE], scalar1=float(S), scalar2=None,
                                                 op0=ALU.add), writes=[idxTf.res])
                DVE.op(lambda h: h.tensor_copy(out=idxT[:], in_=idxTf[:]), reads=[idxTf.res], writes=[idxT.res])
                if debug == "p5":
                    o1 = dout("dbg_idxT", [128, 2, 32], I32)
                    o2 = dout("dbg_valT", [128, 2, 32], F32)
                    o3 = dout("dbg_affT", [64, S // 2], F32)
                    SPQ.dma(lambda h: h.dma_start(out=o1, in_=idxT[:]), reads=[idxT.res])
                    SPQ.dma(lambda h: h.dma_start(out=o2, in_=valT[:]), reads=[valT.res])
                    SPQ.dma(lambda h: h.dma_start(out=o3, in_=affT[:]), reads=[affT.res])

                NGU = 3
                prev_sc = [[], []]
                gu_loaded = [3]

                def ensure_gu(upto):
                    while gu_loaded[0] <= upto and gu_loaded[0] < NE * 4:
                        j = gu_loaded[0]
                        ld_gu(j // 4, j % 4, j % NGU)
                        gu_loaded[0] += 1

                def scatters(e):
                    nonlocal_sc = [[], []]
                    for st in range(4):
                        sq_, hf = st // 2, st % 2
                        row = sq_ * NE + e
                        ye_ = ye[st]
                        for ev in prev_sc[sq_]:
                            POOL.wait_ev(ev)
                        ev = PLQ.dma(lambda h, ye_=ye_, hf=hf, row=row: h.indirect_dma_start(
                            out=out_flat, out_offset=bass.IndirectOffsetOnAxis(ap=idxT[:, hf, row:row + 1], axis=0),
                            in_=ye_[:], in_offset=None, compute_op=ALU.add),
                            reads=[ye_.res, idxT.res])
                        nonlocal_sc[sq_].append(ev)
                    prev_sc[0], prev_sc[1] = nonlocal_sc[0], nonlocal_sc[1]

                gather(0)
                for e in range(NE):
                    xT_, hT_ = xgT[e % 2], hgT[e % 2]
                    for st in range(4):
                        xg_ = xg[st]
                        bk = banks[st % 2]
                        bkb = bk[:].bitcast(BF16)
                        PE.op([(lambda h, dc=dc, xg_=xg_, bkb=bkb: h.transpose(
                            bkb[:, dc * 128:(dc + 1) * 128], xg_[:, dc * 128:(dc + 1) * 128], ident_bf[:]))
                            for dc in range(8)], reads=[xg_.res, ident_bf.res], writes=[bk.res])
                        DVE.op(lambda h, bkb=bkb, xT_=xT_, st=st: h.tensor_copy(
                            out=xT_[:, :, st * 128:(st + 1) * 128], in_=bkb.rearrange("p (c t) -> p c t", c=8)),
                            reads=[bk.res], writes=[xT_.res])
                    if e + 1 < NE:
                        gather(e + 1)
                    for fg in range(4):
                        j = e * 4 + fg
                        ensure_gu(j + NGU - 1)
                        if fg == 1 and e > 0:
                            scatters(e - 1)
                        wg_, wu_ = wgb[j % NGU], wub[j % NGU]
                        for fb in range(4):
                            fi = fg * 4 + fb
                            fs = slice(fb * 128, (fb + 1) * 128)
                            bg, bu = banks[2 + (fi % 2) * 2], banks[3 + (fi % 2) * 2]
                            sl_ = silu[fi % 2]
                            PE.op(mm_group(bg[:], [(wg_[:, dc, fs], xT_[:, dc, :]) for dc in range(8)]),
                                  reads=[wg_.res, xT_.res], writes=[bg.res])
                            PE.op(mm_group(bu[:], [(wu_[:, dc, fs], xT_[:, dc, :]) for dc in range(8)]),
                                  reads=[wu_.res, xT_.res], writes=[bu.res])
                            ACT.op(lambda h, sl_=sl_, bg=bg: h.activation(out=sl_[:], in_=bg[:], func=AF.Silu),
                                   reads=[bg.res], writes=[sl_.res])
                            DVE.op(lambda h, sl_=sl_, bu=bu, fi=fi: h.tensor_tensor(out=hT_[:, fi, :], in0=bu[:], in1=sl_[:], op=ALU.mult),
                                   reads=[bu.res, sl_.res], writes=[hT_.res])
                    for dh in range(2):
                        wd = wdb[dh]
                        for st in range(4):
                            sq_, hf = st // 2, st % 2
                            row = sq_ * NE + e
                            ye_ = ye[st]
                            bk = banks[6 + (st % 2)]
                            PE.op(mm_group(bk[:], [(hT_[:, fi, st * 128:(st + 1) * 128], wd[:, fi, :])
                                                   for fi in range(16)]), reads=[hT_.res, wd.res], writes=[bk.res])
                            if st % 2 == 0:
                                ACT.op(lambda h, bk=bk, ye_=ye_, dh=dh, hf=hf, row=row: h.activation(
                                    out=ye_[:, dh * 512:(dh + 1) * 512], in_=bk[:], func=AF.Copy, scale=valT[:, hf, row:row + 1]),
                                    reads=[bk.res, valT.res], writes=[ye_.res])
                            else:
                                DVE.op(lambda h, bk=bk, ye_=ye_, dh=dh, hf=hf, row=row: h.tensor_scalar(
                                    out=ye_[:, dh * 512:(dh + 1) * 512], in0=bk[:], scalar1=valT[:, hf, row:row + 1], scalar2=None,
                                    op0=ALU.mult), reads=[bk.res, valT.res], writes=[ye_.res])
                        if e + 1 < NE:
                            ld_down(e + 1, dh)
                scatters(NE - 1)
                barrier()
        barrier()
    return nc, dbg


def _rel_bucket_np(rel):
    half, max_exact = 16, 8
    ret = np.where(rel > 0, half, 0)
    n = np.abs(rel)
    nf = np.maximum(n, max_exact).astype(np.float32)
    lg = (np.log(nf / np.float32(max_exact)) / np.float32(math.log(128 / max_exact)) * np.float32(half - max_exact))
    large = max_exact + lg.astype(np.int32)
    large = np.minimum(large, half - 1)
    return ret + np.where(n < max_exact, n, large)


def _host_inputs(inputs):
    f = lambda k: np.ascontiguousarray(np.asarray(inputs[k], dtype=np.float32))
    rel_bias = f("rel_bias")
    kl = np.arange(128)[:, None]
    u = np.arange(STRIP_W)[None, :] - 512
    bidx = _rel_bucket_np(kl - u)
    strip = np.ascontiguousarray(np.transpose(rel_bias[bidx], (2, 0, 1)))
    far = np.stack([rel_bias[31, :], rel_bias[15, :]], axis=1).reshape(1, NH * 2)
    far = np.ascontiguousarray(np.broadcast_to(far, (128, NH * 2)))
    lam4 = np.stack([f("lam_q1")[0], f("lam_k1")[0], f("lam_q2")[0], f("lam_k2")[0]], axis=0)
    pc = lambda a: np.ascontiguousarray(a.reshape(8, 128).T)
    rep2 = lambda a: np.ascontiguousarray(np.concatenate([a, a]).reshape(128, 1))
    conv_w = f("conv_w")[0]
    conv_w_l = np.ascontiguousarray(np.transpose(conv_w.reshape(4, 8, 128), (2, 1, 0)))
    grb, gib = f("gate_r_b")[0], f("gate_i_b")[0]
    gate_b = np.stack([np.stack([pc(grb[0]), pc(grb[1])], axis=1), np.stack([pc(gib[0]), pc(gib[1])], axis=1)], axis=1)
    lam_l = f("lru_lambda")[0]
    lam_l = np.stack([pc(lam_l[0]), pc(lam_l[1])], axis=1)
    w_router_l = np.ascontiguousarray(np.transpose(f("w_router")[0].reshape(8, 128, NE), (1, 0, 2)))
    shared = {
        "g_mix": f("g_mix")[0], "w_in": f("w_in")[0], "g_q": rep2(f("g_q")[0]), "g_k": rep2(f("g_k")[0]),
        "lam4": np.ascontiguousarray(lam4), "g_subln": np.ascontiguousarray(f("g_subln")[0].reshape(128, 1)),
        "bias_strip": strip, "bias_far": far,
        "conv_w": conv_w_l, "conv_b": pc(f("conv_b")[0]), "gate_r_w": f("gate_r_w")[0], "gate_b": np.ascontiguousarray(gate_b),
        "gate_i_w": f("gate_i_w")[0], "lru_lambda": np.ascontiguousarray(lam_l),
        "w_proj_attn": f("w_proj_attn")[0], "w_proj_lru": f("w_proj_lru")[0], "w_out": f("w_out")[0],
        "g_ffn": f("g_ffn")[0], "w_router": w_router_l, "w_gate_e": f("w_gate_e")[0],
        "w_up_e": f("w_up_e")[0], "w_down_e": f("w_down_e")[0],
    }
    x = f("x")
    in_maps = []
    for c in range(NCORES):
        m = dict(shared)
        m["x"] = np.ascontiguousarray(x[c * NSEQ:(c + 1) * NSEQ])
        in_maps.append(m)
    return in_maps


def kernel(**inputs):
    in_maps = _host_inputs(inputs)
    nc, _ = build_program()
    res = run_bass_kernel_spmd(nc, in_maps, core_ids=list(range(NCORES)))
    out = np.concatenate([np.asarray(r["out"]) for r in res.results], axis=0)
    return out.astype(np.float32)
```

```python
import math
import numpy as np
from contextlib import ExitStack
import concourse.bass as bass
import concourse.mybir as mybir
from concourse.bass_utils import run_bass_kernel_spmd

F32 = mybir.dt.float32
BF16 = mybir.dt.bfloat16
U32 = mybir.dt.uint32
I32 = mybir.dt.int32
ALU = mybir.AluOpType
AF = mybir.ActivationFunctionType

NCORES = 8
NSEQ = 2
S = 2048
D = 1024
DIN = 7168
NH = 8
NE = 16
CAP = 256
DFF = 2048
EPS = 1e-6
LAM_INIT = 0.8 - 0.6 * math.exp(-0.3 * 0)
STRIP_W = 1152
NEG_BIG = -1.0e30

SAME_ENGINE_SYNC = True


class Res:
    __slots__ = ("name", "w", "r")

    def __init__(self, name=""):
        self.name = name
        self.w = None
        self.r = {}


class Eng:
    def __init__(self, name, h, sem):
        self.name, self.h, self.sem = name, h, sem
        self.count = 0
        self.waited = {}

    def wait_ev(self, ev):
        if ev is None:
            return
        sem, val = ev
        if sem is self.sem and (not SAME_ENGINE_SYNC or self.name == "pe"):
            return
        if self.waited.get(id(sem), 0) >= val:
            return
        self.h.wait_ge(sem, val)
        self.waited[id(sem)] = val

    def deps(self, reads, writes):
        for r in reads:
            self.wait_ev(r.w)
        for w in writes:
            self.wait_ev(w.w)
            for ev in list(w.r.values()):
                self.wait_ev(ev)

    def op(self, fns, reads=(), writes=()):
        self.deps(reads, writes)
        if not isinstance(fns, (list, tuple)):
            fns = [fns]
        ins = None
        for f in fns:
            ins = f(self.h)
        self.count += 1
        ins.then_inc(self.sem, 1)
        ev = (self.sem, self.count)
        for r in reads:
            r.r[id(self.sem)] = ev
        for w in writes:
            w.w = ev
            w.r = {}
        return ev


class DmaQ:
    def __init__(self, name, eng, sems):
        self.name, self.eng, self.sems = name, eng, sems
        self.n = 0
        self.last = {}

    def dma(self, fn, reads=(), writes=()):
        k = len(self.sems)
        sem = self.sems[self.n % k]
        val = 16 * (self.n // k + 1)
        self.eng.deps(reads, writes)
        if val > 16:
            self.eng.wait_ev((sem, val - 16))
        fn(self.eng.h).then_inc(sem, 16)
        self.n += 1
        ev = (sem, val)
        self.last[id(sem)] = ev
        for r in reads:
            r.r[id(sem)] = ev
        for w in writes:
            w.w = ev
            w.r = {}
        return ev


class T:
    def __init__(self, t, name):
        self.t = t
        self.res = Res(name)

    def __getitem__(self, k):
        return self.t[k]


def build_program(debug=None):
    nc = bass.Bass("TRN2", target_bir_lowering=False)
    dbg = {}

    def din(name, shape, dt=F32):
        return nc.dram_tensor(name, list(shape), dt, kind="ExternalInput").ap()

    x_d = din("x", [NSEQ, S, D])
    g_mix_d = din("g_mix", [D])
    w_in_d = din("w_in", [D, DIN])
    g_q_d = din("g_q", [128, 1])
    g_k_d = din("g_k", [128, 1])
    lam_d = din("lam4", [4, 64])
    g_subln_d = din("g_subln", [128, 1])
    strip_d = din("bias_strip", [NH, 128, STRIP_W])
    far_d = din("bias_far", [128, NH * 2])
    conv_w_d = din("conv_w", [128, 8, 4])
    conv_b_d = din("conv_b", [128, 8])
    gate_r_w_d = din("gate_r_w", [2, 8, 128, 128])
    gate_b_d = din("gate_b", [128, 2, 2, 8])
    gate_i_w_d = din("gate_i_w", [2, 8, 128, 128])
    lru_lambda_d = din("lru_lambda", [128, 2, 8])
    w_pa_d = din("w_proj_attn", [D, D])
    w_pl_d = din("w_proj_lru", [D, D])
    w_out_d = din("w_out", [D, D])
    g_ffn_d = din("g_ffn", [D])
    w_router_d = din("w_router", [128, 8, NE])
    w_gate_d = din("w_gate_e", [NE, D, DFF])
    w_up_d = din("w_up_e", [NE, D, DFF])
    w_down_d = din("w_down_e", [NE, DFF, D])
    out_d = nc.dram_tensor("out", [NSEQ, S, D], F32, kind="ExternalOutput").ap()
    hn_scr = nc.dram_tensor("hn_scr", [NSEQ * S, D], BF16).ap()

    def dout(name, shape, dt=F32):
        a = nc.dram_tensor(name, list(shape), dt, kind="ExternalOutput").ap()
        dbg[name] = a
        return a

    es = ExitStack()
    with es:
        def sem(name):
            return es.enter_context(nc.semaphore(name))

        PE = Eng("pe", nc.tensor, sem("s_pe"))
        ACT = Eng("act", nc.scalar, sem("s_act"))
        DVE = Eng("dve", nc.vector, sem("s_dve"))
        POOL = Eng("pool", nc.gpsimd, sem("s_pool"))
        SPE = Eng("sp", nc.sync, sem("s_sp"))
        ENGS = [PE, ACT, DVE, POOL, SPE]
        SPQ = DmaQ("spq", SPE, [sem(f"s_spq{i}") for i in range(16)])
        PLQ = DmaQ("plq", POOL, [sem(f"s_plq{i}") for i in range(16)])
        QS = [SPQ, PLQ]

        def barrier():
            evs = [(e.sem, e.count) for e in ENGS if e.count > 0]
            for q in QS:
                evs += list(q.last.values())
            for e in ENGS:
                for ev in evs:
                    e.wait_ev(ev)

        uid = [0]

        def sb(stack, name, shape, dt):
            uid[0] += 1
            name = f"{name}_u{uid[0]}"
            return T(stack.enter_context(nc.sbuf_tensor(name, list(shape), dt)), name)

        class TV:
            def __init__(self, ap, name):
                self.ap = ap
                self.res = Res(name)

            def __getitem__(self, k):
                return self.ap[k]

        spair = [es.enter_context(nc.psum_tensor(f"psp{i}", [128, 1024], F32)) for i in range(4)]
        banks = [TV(spair[i // 2][:, (i % 2) * 512:(i % 2 + 1) * 512], f"pb{i}") for i in range(8)]

        ident_bf = sb(es, "ident_bf", [128, 128], BF16)
        ident_f = sb(es, "ident_f", [128, 128], F32)
        ones_bf = sb(es, "ones_bf", [128, 128], BF16)
        ones_f = sb(es, "ones_f", [128, 128], F32)
        blk_bf = sb(es, "blk_bf", [128, 128], BF16)
        cst = Res("consts")
        gmix_b = sb(es, "gmix_b", [128, D], F32)
        gffn_b = sb(es, "gffn_b", [128, D], F32)
        gq2 = sb(es, "gq2", [128, 1], F32)
        gk2 = sb(es, "gk2", [128, 1], F32)
        gsub = sb(es, "gsub", [128, 1], F32)
        lam_t = sb(es, "lam_t", [128, 4 * 64], F32)
        lam_s = sb(es, "lam_s", [128, 4], F32)
        neg_lam = sb(es, "neg_lam", [128, 1], F32)
        far_t = sb(es, "far_t", [128, NH * 2], F32)
        convw = sb(es, "convw", [128, 8, 4], F32)
        convb = sb(es, "convb", [128, 8], F32)
        gbias = sb(es, "gbias", [128, 2, 2, 8], F32)
        nlogc = sb(es, "nlogc", [128, 2, 8], F32)
        nlogc2 = sb(es, "nlogc2", [128, 2, 8], F32)
        gw = sb(es, "gw", [128, 32, 128], BF16)
        wr_bf = sb(es, "wr_bf", [128, 8, NE], BF16)
        aff_all = sb(es, "aff_all", [128, 8, 4 * NE], F32)

        eps64 = sb(es, "eps64", [128, 1], F32)
        eps128 = sb(es, "eps128", [128, 1], F32)
        one_c = sb(es, "one_c", [128, 1], F32)

        def cmem(eng, ap, v):
            eng.op(lambda h: h.memset(ap, v), writes=[cst])

        cmem(DVE, ones_bf[:], 1.0)
        cmem(DVE, ones_f[:], 1.0)
        cmem(DVE, eps64[:], 64.0 * EPS)
        cmem(DVE, eps128[:], 128.0 * EPS)
        cmem(DVE, one_c[:], 1.0)
        cmem(DVE, blk_bf[:], 0.0)
        cmem(DVE, blk_bf[0:64, 0:64], 1.0)
        cmem(DVE, blk_bf[64:128, 64:128], 1.0)
        POOL.op(lambda h: h.affine_select(out=ident_f[:], in_=ones_f[:], pattern=[[1, 128]],
                                          compare_op=ALU.is_equal, fill=0.0, base=0, channel_multiplier=-1),
                reads=[cst], writes=[ident_f.res])
        DVE.op(lambda h: h.tensor_copy(out=ident_bf[:], in_=ident_f[:]), reads=[ident_f.res], writes=[ident_bf.res])

        def cld(q, out_ap, in_ap):
            q.dma(lambda h: h.dma_start(out=out_ap, in_=in_ap), writes=[cst])

        cld(SPQ, gmix_b[:], g_mix_d.partition_broadcast(128))
        cld(SPQ, gffn_b[:], g_ffn_d.partition_broadcast(128))
        cld(SPQ, gq2[:], g_q_d)
        cld(SPQ, gk2[:], g_k_d)
        cld(SPQ, gsub[:], g_subln_d)
        cld(SPQ, lam_t[:], lam_d.rearrange("a d -> (a d)").partition_broadcast(128))
        cld(SPQ, far_t[:], far_d)
        cld(SPQ, convw[:], conv_w_d)
        cld(SPQ, convb[:], conv_b_d)
        cld(SPQ, gbias[:], gate_b_d)
        cld(SPQ, nlogc[:], lru_lambda_d)
        cld(PLQ, gw[:, 0:16, :], gate_r_w_d.rearrange("a n c d -> c (a n) d"))
        cld(PLQ, gw[:, 16:32, :], gate_i_w_d.rearrange("a n c d -> c (a n) d"))
        cld(PLQ, wr_bf[:], w_router_d)

        lamp = lam_t[:].rearrange("p (a d) -> p a d", a=4)
        DVE.op(lambda h: h.tensor_tensor(out=lam_t[:, 0:64], in0=lam_t[:, 0:64], in1=lam_t[:, 64:128], op=ALU.mult),
               reads=[cst], writes=[cst])
        DVE.op(lambda h: h.tensor_tensor(out=lam_t[:, 128:192], in0=lam_t[:, 128:192], in1=lam_t[:, 192:256], op=ALU.mult),
               reads=[cst], writes=[cst])
        DVE.op(lambda h: h.reduce_sum(out=lam_s[:, 0:1], in_=lam_t[:, 0:64], axis=mybir.AxisListType.X),
               reads=[cst], writes=[cst])
        DVE.op(lambda h: h.reduce_sum(out=lam_s[:, 1:2], in_=lam_t[:, 128:192], axis=mybir.AxisListType.X),
               reads=[cst], writes=[cst])
        ACT.op(lambda h: h.activation(out=lam_s[:, 2:4], in_=lam_s[:, 0:2], func=AF.Exp), reads=[cst], writes=[cst])
        DVE.op(lambda h: h.scalar_tensor_tensor(out=neg_lam[:], in0=lam_s[:, 3:4], scalar=-LAM_INIT, in1=lam_s[:, 2:3],
                                                op0=ALU.add, op1=ALU.subtract), reads=[cst], writes=[cst])
        DVE.op(lambda h: h.tensor_scalar(out=gk2[:], in0=gk2[:], scalar1=8.0, scalar2=None, op0=ALU.mult),
               reads=[cst], writes=[cst])
        DVE.op(lambda h: h.tensor_scalar(out=gsub[:], in0=gsub[:], scalar1=math.sqrt(128.0) * (1.0 - LAM_INIT),
                                         scalar2=None, op0=ALU.mult), reads=[cst], writes=[cst])
        ACT.op(lambda h: h.activation(out=nlogc[:], in_=nlogc[:], func=AF.Exp, scale=-1.0), reads=[cst], writes=[cst])
        ACT.op(lambda h: h.activation(out=nlogc[:], in_=nlogc[:], func=AF.Ln, bias=one_c[:, 0:1]), reads=[cst], writes=[cst])
        DVE.op(lambda h: h.tensor_scalar(out=nlogc[:], in0=nlogc[:], scalar1=-8.0, scalar2=None, op0=ALU.mult),
               reads=[cst], writes=[cst])
        DVE.op(lambda h: h.tensor_scalar(out=nlogc2[:], in0=nlogc[:], scalar1=2.0, scalar2=None, op0=ALU.mult),
               reads=[cst], writes=[cst])
        barrier()

        w_in_r = w_in_d.rearrange("(c p) n -> p c n", p=128)

        def mm_group(bank_ap, pairs):
            n = len(pairs)
            return [(lambda h, i=i, l=l, r=r: h.matmul(bank_ap, l, r, start=(i == 0), stop=(i == n - 1)))
                    for i, (l, r) in enumerate(pairs)]

        seq_stack_outer = ExitStack()
        with seq_stack_outer as so:
            xnT = sb(so, "xnT", [128, 8, S], BF16)
            lruT = sb(so, "lruT", [128, 8, S], BF16)
            for s in range(NSEQ):
                with ExitStack() as p1:
                    NXB = 4
                    xb = [sb(p1, f"xb{i}", [128, D], F32) for i in range(NXB)]
                    xnb = [sb(p1, f"xnb{i}", [128, D], BF16) for i in range(NXB)]
                    ssq = [sb(p1, f"ssq{i}", [128, 1], F32) for i in range(NXB)]
                    for it in range(16 + 3):
                        t = it
                        if t < 16:
                            xt, xn, sq = xb[t % NXB], xnb[t % NXB], ssq[t % NXB]
                            SPQ.dma(lambda h, xt=xt, t=t: h.dma_start(out=xt[:], in_=x_d[s, t * 128:(t + 1) * 128, :]),
                                    writes=[xt.res])
                            ACT.op(lambda h, xt=xt, sq=sq, xn=xn: h.activation(out=xn[:], in_=xt[:], func=AF.Square, accum_out=sq[:]),
                                   reads=[xt.res], writes=[xn.res, sq.res])
                        t = it - 1
                        if 0 <= t < 16:
                            sq = ssq[t % NXB]
                            DVE.op(lambda h, sq=sq: h.tensor_scalar(out=sq[:], in0=sq[:], scalar1=1.0 / D, scalar2=EPS,
                                                                    op0=ALU.mult, op1=ALU.add), writes=[sq.res])
                            ACT.op(lambda h, sq=sq: h.activation(out=sq[:], in_=sq[:], func=AF.Ln), writes=[sq.res])
                            ACT.op(lambda h, sq=sq: h.activation(out=sq[:], in_=sq[:], func=AF.Exp, scale=-0.5), writes=[sq.res])
                        t = it - 2
                        if 0 <= t < 16:
                            xt, xn, sq = xb[t % NXB], xnb[t % NXB], ssq[t % NXB]
                            DVE.op(lambda h, xt=xt, xn=xn, sq=sq: h.scalar_tensor_tensor(
                                out=xn[:], in0=xt[:], scalar=sq[:, 0:1], in1=gmix_b[:], op0=ALU.mult, op1=ALU.mult),
                                reads=[xt.res, sq.res, cst], writes=[xn.res])
                        t = it - 3
                        if 0 <= t < 16:
                            xn = xnb[t % NXB]
                            bk = banks[t % 2]
                            bkb = bk[:].bitcast(BF16)
                            PE.op([(lambda h, dc=dc, xn=xn, bkb=bkb: h.transpose(
                                bkb[:, dc * 128:(dc + 1) * 128], xn[:, dc * 128:(dc + 1) * 128], ident_bf[:]))
                                for dc in range(8)], reads=[xn.res, ident_bf.res], writes=[bk.res])
                            if t % 2 == 0:
                                ACT.op(lambda h, bkb=bkb, t=t: h.copy(out=xnT[:, :, t * 128:(t + 1) * 128],
                                                                      in_=bkb.rearrange("p (c t) -> p c t", c=8)),
                                       reads=[bk.res], writes=[xnT.res])
                            else:
                                DVE.op(lambda h, bkb=bkb, t=t: h.tensor_copy(out=xnT[:, :, t * 128:(t + 1) * 128],
                                                                             in_=bkb.rearrange("p (c t) -> p c t", c=8)),
                                       reads=[bk.res], writes=[xnT.res])
                    barrier()
                if debug == "p1" and s == 0:
                    o = dout("dbg_xnT", [128, 8, S], BF16)
                    SPQ.dma(lambda h: h.dma_start(out=o, in_=xnT[:]), reads=[xnT.res])

                if debug in (None, "p2", "p4", "p5"):
                    with ExitStack() as p2:
                        wxb = [sb(p2, f"wx{i}", [128, 8, 128], BF16) for i in range(2)]
                        wyb = [sb(p2, f"wy{i}", [128, 8, 128], BF16) for i in range(2)]
                        xpad2 = [sb(p2, f"xpad{i}", [128, S + 4], F32) for i in range(2)]
                        ybuf = sb(p2, "ybuf", [128, S], F32)
                        xc2 = [sb(p2, f"xc{i}", [128, S], F32) for i in range(2)]
                        xcb2 = [sb(p2, f"xcb{i}", [128, S], BF16) for i in range(2)]
                        rbuf = [sb(p2, f"rbuf{i}", [128, S], F32) for i in range(2)]
                        ibuf = [sb(p2, f"ibuf{i}", [128, S], F32) for i in range(2)]
                        abuf = [sb(p2, f"abuf{i}", [128, S], F32) for i in range(2)]
                        tbuf = [sb(p2, f"tbuf{i}", [128, S], F32) for i in range(2)]
                        for xp_ in xpad2:
                            POOL.op(lambda h, xp_=xp_: h.memset(xp_[:, 0:2], 0.0), writes=[xp_.res])
                            POOL.op(lambda h, xp_=xp_: h.memset(xp_[:, S + 2:S + 4], 0.0), writes=[xp_.res])

                        def ld_lru_w(c):
                            wx, wy = wxb[c % 2], wyb[c % 2]
                            PLQ.dma(lambda h: h.dma_start(out=wx[:], in_=w_in_r[:, :, 3072 + c * 128:3072 + (c + 1) * 128]),
                                    writes=[wx.res])
                            PLQ.dma(lambda h: h.dma_start(out=wy[:], in_=w_in_r[:, :, 4096 + c * 128:4096 + (c + 1) * 128]),
                                    writes=[wy.res])

                        def proj_x(c):
                            wx, xpad = wxb[c % 2], xpad2[c % 2]
                            for tc in range(4):
                                bk = banks[4 + tc]
                                PE.op(mm_group(bk[:], [(wx[:, dc, :], xnT[:, dc, tc * 512:(tc + 1) * 512]) for dc in range(8)]),
                                      reads=[wx.res, xnT.res], writes=[bk.res])
                                ACT.op(lambda h, bk=bk, tc=tc: h.copy(out=xpad[:, 2 + tc * 512:2 + (tc + 1) * 512], in_=bk[:]),
                                       reads=[bk.res], writes=[xpad.res])

                        def conv(c):
                            xpad, xc = xpad2[c % 2], xc2[c % 2]
                            DVE.op(lambda h: h.tensor_scalar(out=xc[:], in0=xpad[:, 0:S], scalar1=convw[:, c, 0:1],
                                                             scalar2=convb[:, c:c + 1], op0=ALU.mult, op1=ALU.add),
                                   reads=[xpad.res, cst], writes=[xc.res])
                            for k in range(1, 4):
                                DVE.op(lambda h, k=k: h.scalar_tensor_tensor(
                                    out=xc[:], in0=xpad[:, k:k + S], scalar=convw[:, c, k:k + 1], in1=xc[:],
                                    op0=ALU.mult, op1=ALU.add), reads=[xpad.res, cst], writes=[xc.res])

                        def cast_x(c):
                            xc, xcb = xc2[c % 2], xcb2[c % 2]
                            ACT.op(lambda h: h.copy(out=xcb[:], in_=xc[:]), reads=[xc.res], writes=[xcb.res])

                        def gates(c, dr):
                            xcb = xcb2[c % 2]
                            for gate, gb in ((0, rbuf[dr]), (1, ibuf[dr])):
                                wi = gate * 16 + dr * 8 + c
                                for pr in range(2):
                                    b0_, b1_ = banks[pr * 2], banks[pr * 2 + 1]
                                    PE.op([lambda h, b0_=b0_, wi=wi, pr=pr: h.matmul(
                                               b0_[:], gw[:, wi, :], xcb[:, pr * 1024:pr * 1024 + 512], start=True, stop=True),
                                           lambda h, b1_=b1_, wi=wi, pr=pr: h.matmul(
                                               b1_[:], gw[:, wi, :], xcb[:, pr * 1024 + 512:(pr + 1) * 1024], start=True, stop=True)],
                                          reads=[xcb.res, cst], writes=[b0_.res, b1_.res])
                                    ACT.op(lambda h, gb=gb, pr=pr, gate=gate: h.activation(
                                        out=gb[:, pr * 1024:(pr + 1) * 1024], in_=spair[pr][:], func=AF.Sigmoid,
                                        bias=gbias[:, gate, dr, c:c + 1]), reads=[b0_.res, b1_.res, cst], writes=[gb.res])

                        def chain(c, dr):
                            rb, ib, ab, tb, xc = rbuf[dr], ibuf[dr], abuf[dr], tbuf[dr], xc2[c % 2]
                            DVE.op(lambda h: h.tensor_tensor(out=ib[:], in0=ib[:], in1=xc[:], op=ALU.mult),
                                   reads=[xc.res], writes=[ib.res])
                            ACT.op(lambda h: h.activation(out=ab[:], in_=rb[:], func=AF.Exp, scale=nlogc[:, dr, c:c + 1]),
                                   reads=[rb.res, cst], writes=[ab.res])
                            ACT.op(lambda h: h.activation(out=tb[:], in_=rb[:], func=AF.Exp, scale=nlogc2[:, dr, c:c + 1]),
                                   reads=[rb.res, cst], writes=[tb.res])
                            ACT.op(lambda h: h.activation(out=tb[:], in_=tb[:], func=AF.Sqrt, scale=-1.0, bias=one_c[:, 0:1]),
                                   reads=[cst], writes=[tb.res])
                            DVE.op(lambda h: h.tensor_tensor(out=ib[:], in0=ib[:], in1=tb[:], op=ALU.mult),
                                   reads=[tb.res], writes=[ib.res])
                            if dr == 0:
                                DVE.op(lambda h: h.tensor_tensor_scan(out=tb[:], data0=ab[:], data1=ib[:], initial=0.0,
                                                                      op0=ALU.mult, op1=ALU.add),
                                       reads=[ab.res, ib.res], writes=[tb.res])
                            else:
                                DVE.op(lambda h: h.tensor_tensor_scan(out=tb[:, ::-1], data0=ab[:, ::-1], data1=ib[:, ::-1],
                                                                      initial=0.0, op0=ALU.mult, op1=ALU.add),
                                       reads=[ab.res, ib.res], writes=[tb.res])

                        ld_lru_w(0)
                        ld_lru_w(1)
                        proj_x(0)
                        conv(0)
                        cast_x(0)
                        for c in range(8):
                            wy = wyb[c % 2]
                            tg_ = abuf[1]
                            gates(c, 0)
                            chain(c, 0)
                            for tc in range(4):
                                bk = banks[4 + tc]
                                PE.op(mm_group(bk[:], [(wy[:, dc, :], xnT[:, dc, tc * 512:(tc + 1) * 512]) for dc in range(8)]),
                                      reads=[wy.res, xnT.res], writes=[bk.res])
                                ACT.op(lambda h, bk=bk, tc=tc: h.copy(out=ybuf[:, tc * 512:(tc + 1) * 512], in_=bk[:]),
                                       reads=[bk.res], writes=[ybuf.res])
                            ACT.op(lambda h: h.activation(out=tg_[:], in_=ybuf[:], func=AF.Square),
                                   reads=[ybuf.res], writes=[tg_.res])
                            DVE.op(lambda h: h.tensor_scalar(out=tg_[:], in0=tg_[:], scalar1=0.044715, scalar2=1.0,
                                                             op0=ALU.mult, op1=ALU.add), writes=[tg_.res])
                            DVE.op(lambda h: h.tensor_tensor(out=tg_[:], in0=tg_[:], in1=ybuf[:], op=ALU.mult),
                                   reads=[ybuf.res], writes=[tg_.res])
                            if c + 1 < 8:
                                proj_x(c + 1)
                                conv(c + 1)
                            ACT.op(lambda h: h.activation(out=tg_[:], in_=tg_[:], func=AF.Sigmoid, scale=1.5957691216057308),
                                   writes=[tg_.res])
                            POOL.op(lambda h: h.tensor_tensor(out=ybuf[:], in0=tg_[:], in1=ybuf[:], op=ALU.mult),
                                    reads=[tg_.res], writes=[ybuf.res])
                            gates(c, 1)
                            chain(c, 1)
                            if c + 1 < 8:
                                cast_x(c + 1)
                            if c + 2 < 8:
                                ld_lru_w(c + 2)
                            POOL.op(lambda h: h.tensor_tensor(out=tbuf[0][:], in0=tbuf[0][:], in1=tbuf[1][:], op=ALU.add),
                                    reads=[tbuf[1].res], writes=[tbuf[0].res])
                            DVE.op(lambda h, c=c: h.tensor_tensor(out=lruT[:, c, :], in0=ybuf[:], in1=tbuf[0][:], op=ALU.mult),
                                   reads=[ybuf.res, tbuf[0].res], writes=[lruT.res])
                        barrier()
                    if debug == "p2" and s == 0:
                        o = dout("dbg_lruT", [128, 8, S], BF16)
                        SPQ.dma(lambda h: h.dma_start(out=o, in_=lruT[:]), reads=[lruT.res])

                p34 = ExitStack()
                oT = sb(p34, "oT", [128, 8, S], BF16)
                if debug in (None, "p3", "p4", "p5"):
                    with ExitStack() as p3:
                        wq = [sb(p3, f"wq{i}", [128, 8, 128], BF16) for i in range(2)]
                        wk = [sb(p3, f"wk{i}", [128, 8, 128], BF16) for i in range(2)]
                        wv = [sb(p3, f"wv{i}", [128, 8, 128], BF16) for i in range(2)]
                        qTh2 = [sb(p3, f"qTh{i}", [128, S], BF16) for i in range(2)]
                        kTz2 = [[sb(p3, f"kTz{i}_{m}", [128, S], BF16) for m in range(2)] for i in range(2)]
                        for i_ in range(2):
                            for m_ in range(2):
                                POOL.op(lambda h, i_=i_, m_=m_: h.memset(kTz2[i_][m_][:], 0.0), writes=[kTz2[i_][m_].res])
                        Vh2 = [sb(p3, f"Vh{i}", [128, 16, 128], BF16) for i in range(2)]
                        sqb = [sb(p3, f"sqb{i}", [128, 512], BF16) for i in range(2)]
                        rsb = [sb(p3, f"rsb{i}", [128, 512], F32) for i in range(2)]
                        NEB = 4
                        Eb = [sb(p3, f"E_{i}", [128, 1024], BF16) for i in range(NEB)]
                        rz = [sb(p3, f"rz{i}", [128, 512], F32) for i in range(2)]
                        t1 = sb(p3, "t1", [128, 512], F32)
                        t2 = sb(p3, "t2", [128, 512], F32)
                        osq = sb(p3, "osq", [128, 512], BF16)
                        ors = rz[0]
                        zacc = [sb(p3, f"zacc{i}", [128, 512], F32) for i in range(2)]
                        zbf = sb(p3, "zbf", [128, 1024], BF16)
                        bO = [banks[4], banks[5]]
                        bA, bB = banks[6], banks[7]
                        strip_f = sb(p3, "strip_f", [128, STRIP_W], F32)
                        strip_hi = [sb(p3, f"strip_hi{i}", [128, STRIP_W], BF16) for i in range(2)]
                        strip_lo = [sb(p3, f"strip_lo{i}", [128, STRIP_W], BF16) for i in range(2)]

                        def ld_attn_w(hd_):
                            SPQ.dma(lambda h: h.dma_start(out=strip_f[:], in_=strip_d[hd_]), writes=[strip_f.res])
                            shi, slo = strip_hi[hd_ % 2], strip_lo[hd_ % 2]
                            DVE.op(lambda h: h.tensor_copy(out=shi[:], in_=strip_f[:]), reads=[strip_f.res], writes=[shi.res])
                            DVE.op(lambda h: h.tensor_tensor(out=slo[:], in0=strip_f[:], in1=shi[:], op=ALU.subtract),
                                   reads=[strip_f.res, shi.res], writes=[slo.res])
                            for (wt, off) in ((wq[hd_ % 2], 0), (wk[hd_ % 2], 1024), (wv[hd_ % 2], 2048)):
                                PLQ.dma(lambda h, wt=wt, off=off: h.dma_start(
                                    out=wt[:], in_=w_in_r[:, :, off + hd_ * 128:off + (hd_ + 1) * 128]), writes=[wt.res])

                        bB_lock = [None]
                        DONE = object()

                        def acquire(me):
                            while bB_lock[0] not in (None, me):
                                yield
                            bB_lock[0] = me

                        class BG:
                            fin = []
                            gen = None

                            @staticmethod
                            def step():
                                if BG.fin:
                                    if next(BG.fin[0], DONE) is DONE:
                                        BG.fin.pop(0)
                                if BG.gen is not None:
                                    if next(BG.gen, DONE) is DONE:
                                        BG.gen = None

                            @staticmethod
                            def drain_gen():
                                while BG.gen is not None:
                                    BG.step()

                            @staticmethod
                            def drain_all():
                                while BG.gen is not None or BG.fin:
                                    BG.step()

                        BG.fin = []
                        BG.gen = None

                        def proj_steps(hd_):
                            qT_, kT_, V_ = qTh2[hd_ % 2], kTz2[hd_ % 2], Vh2[hd_ % 2]
                            i2 = 0
                            for (wt, dst, gsc) in ((wq[hd_ % 2], qT_, gq2), (wk[hd_ % 2], kT_, gk2)):
                                for tc in range(4):
                                    sq_, rs_ = sqb[i2 % 2], rsb[i2 % 2]
                                    i2 += 1
                                    PE.op(mm_group(bA[:], [(wt[:, dc, :], xnT[:, dc, tc * 512:(tc + 1) * 512]) for dc in range(8)]),
                                          reads=[wt.res, xnT.res], writes=[bA.res])
                                    yield
                                    ACT.op(lambda h, sq_=sq_: h.activation(out=sq_[:], in_=bA[:], func=AF.Square),
                                           reads=[bA.res], writes=[sq_.res])
                                    yield
                                    yield from acquire("p")
                                    PE.op(lambda h, sq_=sq_: h.matmul(bB[:], blk_bf[:], sq_[:], start=True, stop=True),
                                          reads=[sq_.res, cst], writes=[bB.res])
                                    yield
                                    ACT.op(lambda h, rs_=rs_: h.activation(out=rs_[:], in_=bB[:], func=AF.Ln, bias=eps64[:, 0:1]),
                                           reads=[bB.res, cst], writes=[rs_.res])
                                    ACT.op(lambda h, rs_=rs_: h.activation(out=rs_[:], in_=rs_[:], func=AF.Exp, scale=-0.5),
                                           writes=[rs_.res])
                                    bB_lock[0] = None
                                    yield
                                    if isinstance(dst, list):
                                        for m_ in range(2):
                                            ps_ = slice(m_ * 64, (m_ + 1) * 64)
                                            DVE.op(lambda h, rs_=rs_, dst=dst, gsc=gsc, tc=tc, m_=m_, ps_=ps_: h.scalar_tensor_tensor(
                                                out=dst[m_][ps_, tc * 512:(tc + 1) * 512], in0=bA[ps_, :], scalar=gsc[ps_, 0:1],
                                                in1=rs_[ps_, :], op0=ALU.mult, op1=ALU.mult),
                                                reads=[bA.res, rs_.res, cst], writes=[dst[m_].res])
                                    else:
                                        DVE.op(lambda h, rs_=rs_, dst=dst, gsc=gsc, tc=tc: h.scalar_tensor_tensor(
                                            out=dst[:, tc * 512:(tc + 1) * 512], in0=bA[:], scalar=gsc[:, 0:1], in1=rs_[:],
                                            op0=ALU.mult, op1=ALU.mult), reads=[bA.res, rs_.res, cst], writes=[dst.res])
                                    yield
                            wv_ = wv[hd_ % 2]
                            for tg in range(4):
                                fl = []
                                for j in range(4):
                                    tt = tg * 4 + j
                                    fl += mm_group(bA[:, j * 128:(j + 1) * 128],
                                                   [(xnT[:, dc, tt * 128:(tt + 1) * 128], wv_[:, dc, :]) for dc in range(8)])
                                PE.op(fl, reads=[wv_.res, xnT.res], writes=[bA.res])
                                yield
                                ACT.op(lambda h, tg=tg: h.copy(out=V_[:, tg * 4:(tg + 1) * 4, :],
                                                               in_=bA[:].rearrange("p (j v) -> p j v", j=4)),
                                       reads=[bA.res], writes=[V_.res])
                                yield

                        def fin_steps(hd_, qs):
                            for m in range(2):
                                yield from acquire("f")
                                PE.op(lambda h, m=m: h.matmul(bB[:], ones_bf[:], zbf[:, m * 512:(m + 1) * 512], start=True, stop=True),
                                      reads=[zbf.res, cst], writes=[bB.res])
                                yield
                                ACT.op(lambda h, m=m: h.activation(out=rz[m][:], in_=bB[:], func=AF.Ln),
                                       reads=[bB.res], writes=[rz[m].res])
                                ACT.op(lambda h, m=m: h.activation(out=rz[m][:], in_=rz[m][:], func=AF.Exp, scale=-1.0),
                                       writes=[rz[m].res])
                                bB_lock[0] = None
                                yield
                            DVE.op(lambda h: h.tensor_tensor(out=t1[:], in0=t1[:], in1=rz[0][:], op=ALU.mult),
                                   reads=[rz[0].res], writes=[t1.res])
                            DVE.op(lambda h: h.tensor_tensor(out=t2[:], in0=t2[:], in1=rz[1][:], op=ALU.mult),
                                   reads=[rz[1].res], writes=[t2.res])
                            DVE.op(lambda h: h.scalar_tensor_tensor(out=t1[:], in0=t2[:], scalar=neg_lam[:, 0:1], in1=t1[:],
                                                                    op0=ALU.mult, op1=ALU.add),
                                   reads=[t2.res, cst], writes=[t1.res])
                            yield
                            ACT.op(lambda h: h.activation(out=osq[:], in_=t1[:], func=AF.Square),
                                   reads=[t1.res], writes=[osq.res])
                            yield
                            yield from acquire("f")
                            PE.op(lambda h: h.matmul(bB[:], ones_bf[:], osq[:], start=True, stop=True),
                                  reads=[osq.res, cst], writes=[bB.res])
                            yield
                            ACT.op(lambda h: h.activation(out=ors[:], in_=bB[:], func=AF.Ln, bias=eps128[:, 0:1]),
                                   reads=[bB.res, cst], writes=[ors.res])
                            ACT.op(lambda h: h.activation(out=ors[:], in_=ors[:], func=AF.Exp, scale=-0.5), writes=[ors.res])
                            bB_lock[0] = None
                            yield
                            DVE.op(lambda h: h.scalar_tensor_tensor(out=oT[:, hd_, qs], in0=t1[:], scalar=gsub[:, 0:1], in1=ors[:],
                                                                    op0=ALU.mult, op1=ALU.mult),
                                   reads=[t1.res, ors.res, cst], writes=[oT.res])
                            yield

                        ld_attn_w(0)
                        BG.gen = proj_steps(0)
                        BG.drain_gen()
                        ecnt = 0
                        for hd_ in range(NH):
                            if hd_ + 1 < NH:
                                ld_attn_w(hd_ + 1)
                                BG.gen = proj_steps(hd_ + 1)
                            qTh, kTz, Vh = qTh2[hd_ % 2], kTz2[hd_ % 2], Vh2[hd_ % 2]
                            for qc in range(4):
                                qs = slice(qc * 512, (qc + 1) * 512)

                                def is_far(kc):
                                    delta = kc * 128 - qc * 512
                                    return delta >= 602 or delta <= -218

                                def emit_S(kc):
                                    p_ = kc % 2
                                    delta = kc * 128 - qc * 512
                                    for m in range(2):
                                        bk_ = banks[p_ * 2 + m]
                                        if is_far(kc):
                                            PE.op(lambda h, m=m, kc=kc, bk_=bk_: h.matmul(
                                                bk_[:], kTz[m][:, kc * 128:(kc + 1) * 128], qTh[:, qs], start=True, stop=True),
                                                reads=[kTz[m].res, qTh.res], writes=[bk_.res])
                                        else:
                                            u0 = 512 - delta
                                            shi, slo = strip_hi[hd_ % 2], strip_lo[hd_ % 2]
                                            PE.op([lambda h, m=m, kc=kc, bk_=bk_: h.matmul(
                                                       bk_[:], kTz[m][:, kc * 128:(kc + 1) * 128], qTh[:, qs], start=True, stop=False),
                                                   lambda h, bk_=bk_, u0=u0, shi=shi: h.matmul(
                                                       bk_[:], ident_bf[:], shi[:, u0:u0 + 512], start=False, stop=False),
                                                   lambda h, bk_=bk_, u0=u0, slo=slo: h.matmul(
                                                       bk_[:], ident_bf[:], slo[:, u0:u0 + 512], start=False, stop=True)],
                                                  reads=[kTz[m].res, qTh.res, shi.res, slo.res, ident_bf.res], writes=[bk_.res])

                                def emit_exp(kc):
                                    nonlocal ecnt
                                    delta = kc * 128 - qc * 512
                                    p_ = kc % 2
                                    b0_, b1_ = banks[p_ * 2], banks[p_ * 2 + 1]
                                    E_ = Eb[ecnt % NEB]
                                    ecnt += 1
                                    if is_far(kc):
                                        col = hd_ * 2 + (0 if delta > 0 else 1)
                                        ACT.op(lambda h, E_=E_, col=col, p_=p_: h.activation(
                                            out=E_[:], in_=spair[p_][:], func=AF.Exp, bias=far_t[:, col:col + 1]),
                                            reads=[b0_.res, b1_.res, cst], writes=[E_.res])
                                    else:
                                        ACT.op(lambda h, E_=E_, p_=p_: h.activation(out=E_[:], in_=spair[p_][:], func=AF.Exp),
                                               reads=[b0_.res, b1_.res], writes=[E_.res])
                                    return E_

                                def emit_PV(kc, E_):
                                    PE.op([lambda h, kc=kc: h.matmul(bO[0][:], Vh[:, kc, :], E_[:, 0:512],
                                                                     start=(kc == 0), stop=(kc == 15)),
                                           lambda h, kc=kc: h.matmul(bO[1][:], Vh[:, kc, :], E_[:, 512:1024],
                                                                     start=(kc == 0), stop=(kc == 15))],
                                          reads=[Vh.res, E_.res], writes=[bO[0].res, bO[1].res])
                                    for m, eng in ((0, POOL), (1, DVE)):
                                        if kc == 0:
                                            eng.op(lambda h, m=m: h.tensor_copy(out=zacc[m][:], in_=E_[:, m * 512:(m + 1) * 512]),
                                                   reads=[E_.res], writes=[zacc[m].res])
                                        else:
                                            eng.op(lambda h, m=m: h.tensor_tensor(out=zacc[m][:], in0=zacc[m][:],
                                                                                  in1=E_[:, m * 512:(m + 1) * 512], op=ALU.add),
                                                   reads=[E_.res], writes=[zacc[m].res])

                                emit_S(0)
                                emit_S(1)
                                prev = None
                                for kc in range(16):
                                    Es = emit_exp(kc)
                                    if prev is not None:
                                        emit_PV(kc - 1, prev)
                                    if kc + 2 < 16:
                                        emit_S(kc + 2)
                                    prev = Es
                                    BG.step()
                                emit_PV(15, prev)
                                ACT.op(lambda h: h.copy(out=t1[:], in_=bO[0][:]), reads=[bO[0].res], writes=[t1.res])
                                DVE.op(lambda h: h.tensor_copy(out=t2[:], in_=bO[1][:]), reads=[bO[1].res], writes=[t2.res])
                                for m in range(2):
                                    DVE.op(lambda h, m=m: h.tensor_copy(out=zbf[:, m * 512:(m + 1) * 512], in_=zacc[m][:]),
                                           reads=[zacc[m].res], writes=[zbf.res])
                                BG.fin.append(fin_steps(hd_, qs))
                            BG.drain_gen()
                        BG.drain_all()
                        barrier()
                    if debug == "p3" and s == 0:
                        o = dout("dbg_oT", [128, 8, S], BF16)
                        SPQ.dma(lambda h: h.dma_start(out=o, in_=oT[:]), reads=[oT.res])

                if debug in (None, "p4", "p5"):
                    with ExitStack() as p4:
                        NTH = 2
                        TH = S // NTH
                        wpc = [[sb(p4, f"wpc{k}_{i}", [128, 8, 128], BF16) for i in range(2)] for k in range(4)]
                        wo = sb(p4, "wo", [128, 8, D], BF16)
                        w_pa_r = w_pa_d.rearrange("(c p) n -> p c n", p=128)
                        w_pl_r = w_pl_d.rearrange("(c p) n -> p c n", p=128)
                        w_out_r = w_out_d.rearrange("(c p) n -> p c n", p=128)
                        for half in range(2):
                            PLQ.dma(lambda h, half=half: h.dma_start(
                                out=wo[:, half * 4:(half + 1) * 4, :], in_=w_out_r[:, half * 4:(half + 1) * 4, :]),
                                writes=[wo.res])
                        sg = [sb(p4, f"sg{i}", [128, 512], BF16) for i in range(4)]
                        mixA = [sb(p4, f"mixA{i}", [128, 512], F32) for i in range(2)]
                        mixT = sb(p4, "mixT", [128, 8, TH], BF16)
                        hbuf = [sb(p4, f"hbuf{i}", [128, D], F32) for i in range(4)]
                        hnb = [sb(p4, f"hnb{i}", [128, D], BF16) for i in range(3)]
                        ssq4 = [sb(p4, f"ssq4{i}", [128, 1], F32) for i in range(4)]
                        hnT = [sb(p4, f"hnT{i}", [128, 8, 128], BF16) for i in range(2)]
                        lg = [sb(p4, f"lg{i}", [128, NE], F32) for i in range(2)]
                        lsum = [sb(p4, f"lsum{i}", [128, 1], F32) for i in range(2)]
                        hres = [Res(f"out_rows{s}")]

                        def ld_p4_w(i):
                            db = i % 8
                            dsl = slice(db * 128, (db + 1) * 128)
                            srcs = (w_in_r[:, :, 5120 + db * 128:5120 + (db + 1) * 128],
                                    w_in_r[:, :, 6144 + db * 128:6144 + (db + 1) * 128],
                                    w_pa_r[:, :, dsl], w_pl_r[:, :, dsl])
                            for k in range(4):
                                wt = wpc[k][i % 2]
                                PLQ.dma(lambda h, wt=wt, k=k: h.dma_start(out=wt[:], in_=srcs[k]), writes=[wt.res])

                        ld_p4_w(0)
                        sgc = 0
                        itc = 0
                        for th in range(NTH):
                            for db in range(8):
                                i4 = th * 8 + db
                                if i4 + 1 < NTH * 8:
                                    ld_p4_w(i4 + 1)
                                wga_, wgr_, wpa_, wpl_ = (wpc[k][i4 % 2] for k in range(4))
                                for tcl in range(TH // 512):
                                    tc = th * (TH // 512) + tcl
                                    ts_ = slice(tc * 512, (tc + 1) * 512)
                                    sga, sgr = sg[sgc % 4], sg[(sgc + 1) % 4]
                                    sgc += 2
                                    mA = mixA[itc % 2]
                                    bo = (itc % 2) * 4
                                    itc += 1
                                    b0, b1, b2, b3 = banks[bo], banks[bo + 1], banks[bo + 2], banks[bo + 3]
                                    PE.op(mm_group(b0[:], [(wga_[:, dc, :], xnT[:, dc, ts_]) for dc in range(8)]),
                                          reads=[wga_.res, xnT.res], writes=[b0.res])
                                    ACT.op(lambda h, sga=sga, b0=b0: h.activation(out=sga[:], in_=b0[:], func=AF.Sigmoid),
                                           reads=[b0.res], writes=[sga.res])
                                    PE.op(mm_group(b1[:], [(wgr_[:, dc, :], xnT[:, dc, ts_]) for dc in range(8)]),
                                          reads=[wgr_.res, xnT.res], writes=[b1.res])
                                    ACT.op(lambda h, sgr=sgr, b1=b1: h.activation(out=sgr[:], in_=b1[:], func=AF.Sigmoid),
                                           reads=[b1.res], writes=[sgr.res])
                                    PE.op(mm_group(b2[:], [(wpa_[:, ac, :], oT[:, ac, ts_]) for ac in range(8)]),
                                          reads=[wpa_.res, oT.res], writes=[b2.res])
                                    DVE.op(lambda h, mA=mA, sga=sga, b2=b2: h.tensor_tensor(out=mA[:], in0=b2[:], in1=sga[:], op=ALU.mult),
                                           reads=[b2.res, sga.res], writes=[mA.res])
                                    PE.op(mm_group(b3[:], [(wpl_[:, ac, :], lruT[:, ac, ts_]) for ac in range(8)]),
                                          reads=[wpl_.res, lruT.res], writes=[b3.res])
                                    DVE.op(lambda h, sgr=sgr, b3=b3: h.tensor_tensor(out=sgr[:], in0=b3[:], in1=sgr[:], op=ALU.mult),
                                           reads=[b3.res], writes=[sgr.res])
                                    POOL.op(lambda h, mA=mA, sgr=sgr, db=db, tcl=tcl: h.tensor_tensor(
                                        out=mixT[:, db, tcl * 512:(tcl + 1) * 512], in0=mA[:], in1=sgr[:], op=ALU.add),
                                        reads=[mA.res, sgr.res], writes=[mixT.res])
                            NTT = TH // 128
                            for it4 in range(NTT + 4):
                                ttl = it4
                                if ttl < NTT:
                                    gt = th * NTT + ttl
                                    rows = slice(gt * 128, (gt + 1) * 128)
                                    hb = hbuf[gt % 4]
                                    SPQ.dma(lambda h, hb=hb, rows=rows: h.dma_start(out=hb[:], in_=x_d[s, rows, :]), writes=[hb.res])
                                    for eh in range(2):
                                        bk = banks[(gt % 2) * 4 + eh]
                                        PE.op(mm_group(bk[:], [(mixT[:, dc, ttl * 128:(ttl + 1) * 128], wo[:, dc, eh * 512:(eh + 1) * 512])
                                                               for dc in range(8)]), reads=[mixT.res, wo.res], writes=[bk.res])
                                        DVE.op(lambda h, bk=bk, hb=hb, eh=eh: h.tensor_tensor(
                                            out=hb[:, eh * 512:(eh + 1) * 512], in0=bk[:], in1=hb[:, eh * 512:(eh + 1) * 512], op=ALU.add),
                                            reads=[bk.res], writes=[hb.res])
                                ttl = it4 - 1
                                if 0 <= ttl < NTT:
                                    gt = th * NTT + ttl
                                    rows = slice(gt * 128, (gt + 1) * 128)
                                    hb, hn, sq4 = hbuf[gt % 4], hnb[gt % 3], ssq4[gt % 4]
                                    SPQ.dma(lambda h, hb=hb, rows=rows: h.dma_start(out=out_d[s, rows, :], in_=hb[:]),
                                            reads=[hb.res], writes=[hres[0]])
                                    ACT.op(lambda h, hb=hb, sq4=sq4, hn=hn: h.activation(out=hn[:], in_=hb[:], func=AF.Square, accum_out=sq4[:]),
                                           reads=[hb.res], writes=[hn.res, sq4.res])
                                ttl = it4 - 2
                                if 0 <= ttl < NTT:
                                    gt = th * NTT + ttl
                                    hb, hn, sq4 = hbuf[gt % 4], hnb[gt % 3], ssq4[gt % 4]
                                    DVE.op(lambda h, sq4=sq4: h.tensor_scalar(out=sq4[:], in0=sq4[:], scalar1=1.0 / D, scalar2=EPS,
                                                                              op0=ALU.mult, op1=ALU.add), writes=[sq4.res])
                                    ACT.op(lambda h, sq4=sq4: h.activation(out=sq4[:], in_=sq4[:], func=AF.Ln), writes=[sq4.res])
                                    ACT.op(lambda h, sq4=sq4: h.activation(out=sq4[:], in_=sq4[:], func=AF.Exp, scale=-0.5), writes=[sq4.res])
                                    DVE.op(lambda h, hb=hb, hn=hn, sq4=sq4: h.scalar_tensor_tensor(
                                        out=hn[:], in0=hb[:], scalar=sq4[:, 0:1], in1=gffn_b[:], op0=ALU.mult, op1=ALU.mult),
                                        reads=[hb.res, sq4.res, cst], writes=[hn.res])
                                    SPQ.dma(lambda h, hn=hn, gt=gt: h.dma_start(
                                        out=hn_scr[s * S + gt * 128:s * S + (gt + 1) * 128, :], in_=hn[:]),
                                        reads=[hn.res], writes=[hres[0]])
                                ttl = it4 - 3
                                if 0 <= ttl < NTT:
                                    gt = th * NTT + ttl
                                    hn, hT = hnb[gt % 3], hnT[gt % 2]
                                    bk = banks[2 + (gt % 2) * 4]
                                    bkb = bk[:].bitcast(BF16)
                                    PE.op([(lambda h, dc=dc, hn=hn, bkb=bkb: h.transpose(
                                        bkb[:, dc * 128:(dc + 1) * 128], hn[:, dc * 128:(dc + 1) * 128], ident_bf[:]))
                                        for dc in range(8)], reads=[hn.res, ident_bf.res], writes=[bk.res])
                                    ACT.op(lambda h, hT=hT, bkb=bkb: h.copy(out=hT[:], in_=bkb.rearrange("p (c t) -> p c t", c=8)),
                                           reads=[bk.res], writes=[hT.res])
                                ttl = it4 - 4
                                if 0 <= ttl < NTT:
                                    gt = th * NTT + ttl
                                    hT, lg_, ls_ = hnT[gt % 2], lg[gt % 2], lsum[gt % 2]
                                    bk7 = banks[3 + (gt % 2) * 4]
                                    PE.op(mm_group(bk7[:, 0:NE], [(hT[:, dc, :], wr_bf[:, dc, :]) for dc in range(8)]),
                                          reads=[hT.res, cst], writes=[bk7.res])
                                    ACT.op(lambda h, lg_=lg_, ls_=ls_, bk7=bk7: h.activation(out=lg_[:], in_=bk7[:, 0:NE], func=AF.Exp,
                                                                                             accum_out=ls_[:]),
                                           reads=[bk7.res], writes=[lg_.res, ls_.res])
                                    DVE.op(lambda h, ls_=ls_: h.reciprocal(out=ls_[:], in_=ls_[:]), writes=[ls_.res])
                                    DVE.op(lambda h, lg_=lg_, ls_=ls_, gt=gt: h.tensor_scalar(
                                        out=aff_all[:, gt % 8, (gt // 8) * 2 * NE + s * NE:(gt // 8) * 2 * NE + (s + 1) * NE], in0=lg_[:], scalar1=ls_[:, 0:1], scalar2=None,
                                        op0=ALU.mult), reads=[lg_.res, ls_.res], writes=[cst])
                        barrier()
                    if debug == "p4" and s == 0:
                        barrier()
                p34.close()
            barrier()

        if debug in (None, "p5"):
            with ExitStack() as p5:
                HS = S // 2
                affT = sb(p5, "affT", [64, HS], F32)
                work = sb(p5, "work", [64, HS], F32)
                hvals = sb(p5, "hvals", [64, CAP], F32)
                idxu = sb(p5, "idxu", [64, CAP], U32)
                hidx = sb(p5, "hidx", [64, CAP], F32)
                bshv = sb(p5, "bshv", [32, CAP], F32)
                bshi = sb(p5, "bshi", [32, CAP], F32)
                msk = sb(p5, "msk", [32, CAP], U32)
                vals = sb(p5, "vals", [32, CAP], F32)
                idxf = sb(p5, "idxf", [32, CAP], F32)
                idxT = sb(p5, "idxT", [128, 2, 32], I32)
                idxTf = sb(p5, "idxTf", [128, 2, 32], F32)
                valT = sb(p5, "valT", [128, 2, 32], F32)
                wgb = [sb(p5, f"wgb{i}", [128, 8, 512], BF16) for i in range(3)]
                wub = [sb(p5, f"wub{i}", [128, 8, 512], BF16) for i in range(3)]
                wdb = [sb(p5, f"wdb{i}", [128, 16, 512], BF16) for i in range(2)]
                xg = [sb(p5, f"xg{i}", [128, D], BF16) for i in range(4)]
                xgT = [sb(p5, f"xgT{i}", [128, 8, 512], BF16) for i in range(2)]
                hgT = [sb(p5, f"hgT{i}", [128, 16, 512], BF16) for i in range(2)]
                silu = [sb(p5, f"silu{i}", [128, 512], F32) for i in range(2)]
                ye = [sb(p5, f"ye{i}", [128, D], F32) for i in range(4)]
                w_gate_r = w_gate_d.rearrange("e (c p) f -> e p c f", p=128)
                w_up_r = w_up_d.rearrange("e (c p) f -> e p c f", p=128)
                w_down_r = w_down_d.rearrange("e (c p) d -> e p c d", p=128)
                out_flat = out_d.rearrange("s t d -> (s t) d")

                def ld_gu(e, fg, slot):
                    PLQ.dma(lambda h: h.dma_start(out=wgb[slot][:], in_=w_gate_r[e, :, :, fg * 512:(fg + 1) * 512]),
                            writes=[wgb[slot].res])
                    PLQ.dma(lambda h: h.dma_start(out=wub[slot][:], in_=w_up_r[e, :, :, fg * 512:(fg + 1) * 512]),
                            writes=[wub[slot].res])

                def ld_down(e, dh):
                    wd = wdb[dh]
                    for q2 in range(2):
                        PLQ.dma(lambda h, q2=q2: h.dma_start(out=wd[:, q2 * 8:(q2 + 1) * 8, :],
                                                             in_=w_down_r[e, :, q2 * 8:(q2 + 1) * 8, dh * 512:(dh + 1) * 512]),
                                writes=[wd.res])

                def gather(e):
                    for st in range(4):
                        sq_, hf = st // 2, st % 2
                        row = sq_ * NE + e
                        xg_ = xg[st]
                        PLQ.dma(lambda h, xg_=xg_, hf=hf, row=row: h.indirect_dma_start(
                            out=xg_[:], out_offset=None, in_=hn_scr,
                            in_offset=bass.IndirectOffsetOnAxis(ap=idxT[:, hf, row:row + 1], axis=0)),
                            reads=[idxT.res], writes=[xg_.res])

                for j_ in range(3):
                    ld_gu(0, j_, j_)
                ld_down(0, 0)
                ld_down(0, 1)
                for g in range(2):
                    bk = banks[g]
                    PE.op([(lambda h, j=j, bk=bk, g=g: h.transpose(bk[0:64, j * 128:(j + 1) * 128], aff_all[:, g * 4 + j, :], ident_f[:]))
                           for j in range(4)], reads=[cst, ident_f.res], writes=[bk.res])
                    DVE.op(lambda h, bk=bk, g=g: h.tensor_copy(out=affT[:, g * 512:(g + 1) * 512], in_=bk[0:64, :]),
                           reads=[bk.res], writes=[affT.res])
                for r in range(CAP // 8):
                    src = affT if r == 0 else work
                    DVE.op(lambda h, r=r, src=src: h.max(out=hvals[:, r * 8:(r + 1) * 8], in_=src[:]),
                           reads=[src.res], writes=[hvals.res])
                    DVE.op(lambda h, r=r, src=src: h.max_index(out=idxu[:, r * 8:(r + 1) * 8], in_max=hvals[:, r * 8:(r + 1) * 8],
                                                             in_values=src[:]), reads=[src.res, hvals.res], writes=[idxu.res])
                    if r + 1 < CAP // 8:
                        DVE.op(lambda h, r=r, src=src: h.match_replace(out=work[:], in_to_replace=hvals[:, r * 8:(r + 1) * 8],
                                                                     in_values=src[:], imm_value=NEG_BIG),
                               reads=[src.res, hvals.res], writes=[work.res])
                DVE.op(lambda h: h.tensor_copy(out=hidx[:], in_=idxu[:]), reads=[idxu.res], writes=[hidx.res])
                DVE.op(lambda h: h.tensor_scalar(out=hidx[32:64, :], in0=hidx[32:64, :], scalar1=float(HS), scalar2=None, op0=ALU.add),
                       writes=[hidx.res])
                SPQ.dma(lambda h: h.dma_start(out=bshv[:], in_=hvals[32:64, :]), reads=[hvals.res], writes=[bshv.res])
                SPQ.dma(lambda h: h.dma_start(out=bshi[:], in_=hidx[32:64, :]), reads=[hidx.res], writes=[bshi.res])
                DVE.op(lambda h: h.tensor_tensor(out=msk[:], in0=hvals[0:32, :], in1=bshv[:, ::-1], op=ALU.is_gt),
                       reads=[hvals.res, bshv.res], writes=[msk.res])
                DVE.op(lambda h: h.tensor_tensor(out=vals[:], in0=hvals[0:32, :], in1=bshv[:, ::-1], op=ALU.max),
                       reads=[hvals.res, bshv.res], writes=[vals.res])
                DVE.op(lambda h: h.tensor_copy(out=idxf[:], in_=bshi[:, ::-1]), reads=[bshi.res], writes=[idxf.res])
                DVE.op(lambda h: h.copy_predicated(out=idxf[:], mask=msk[:], data=hidx[0:32, :]),
                       reads=[msk.res, hidx.res], writes=[idxf.res])
                for hf in range(2):
                    bk = banks[2]
                    PE.op(lambda h, hf=hf, bk=bk: h.transpose(bk[:, 0:32], idxf[:, hf * 128:(hf + 1) * 128], ident_f[0:32, 0:32]),
                          reads=[idxf.res, ident_f.res], writes=[bk.res])
                    DVE.op(lambda h, hf=hf, bk=bk: h.tensor_copy(out=idxTf[:, hf, :], in_=bk[:, 0:32]),
                           reads=[bk.res], writes=[idxTf.res])
                    bk = banks[3]
                    PE.op(lambda h, hf=hf, bk=bk: h.transpose(bk[:, 0:32], vals[:, hf * 128:(hf + 1) * 128], ident_f[0:32, 0:32]),
                          reads=[vals.res, ident_f.res], writes=[bk.res])
                    DVE.op(lambda h, hf=hf, bk=bk: h.tensor_copy(out=valT[:, hf, :], in_=bk[:, 0:32]),
                           reads=[bk.res], writes=[valT.res])
                DVE.op(lambda h: h.tensor_scalar(out=idxTf[:, :, NE:2 * NE], in0=idxTf[:, :, NE:2 * NE], scalar1=float(S), scalar2=None,
                                                 op0=ALU.add), writes=[idxTf.res])
                DVE.op(lambda h: h.tensor_copy(out=idxT[:], in_=idxTf[:]), reads=[idxTf.res], writes=[idxT.res])
                if debug == "p5":
                    o1 = dout("dbg_idxT", [128, 2, 32], I32)
                    o2 = dout("dbg_valT", [128, 2, 32], F32)
                    o3 = dout("dbg_affT", [64, S // 2], F32)
                    SPQ.dma(lambda h: h.dma_start(out=o1, in_=idxT[:]), reads=[idxT.res])
                    SPQ.dma(lambda h: h.dma_start(out=o2, in_=valT[:]), reads=[valT.res])
                    SPQ.dma(lambda h: h.dma_start(out=o3, in_=affT[:]), reads=[affT.res])

                NGU = 3
                prev_sc = [[], []]
                gu_loaded = [3]

                def ensure_gu(upto):
                    while gu_loaded[0] <= upto and gu_loaded[0] < NE * 4:
                        j = gu_loaded[0]
                        ld_gu(j // 4, j % 4, j % NGU)
                        gu_loaded[0] += 1

                def scatters(e):
                    nonlocal_sc = [[], []]
                    for st in range(4):
                        sq_, hf = st // 2, st % 2
                        row = sq_ * NE + e
                        ye_ = ye[st]
                        for ev in prev_sc[sq_]:
                            POOL.wait_ev(ev)
                        ev = PLQ.dma(lambda h, ye_=ye_, hf=hf, row=row: h.indirect_dma_start(
                            out=out_flat, out_offset=bass.IndirectOffsetOnAxis(ap=idxT[:, hf, row:row + 1], axis=0),
                            in_=ye_[:], in_offset=None, compute_op=ALU.add),
                            reads=[ye_.res, idxT.res])
                        nonlocal_sc[sq_].append(ev)
                    prev_sc[0], prev_sc[1] = nonlocal_sc[0], nonlocal_sc[1]

                gather(0)
                for e in range(NE):
                    xT_, hT_ = xgT[e % 2], hgT[e % 2]
                    for st in range(4):
                        xg_ = xg[st]
                        bk = banks[st % 2]
                        bkb = bk[:].bitcast(BF16)
                        PE.op([(lambda h, dc=dc, xg_=xg_, bkb=bkb: h.transpose(
                            bkb[:, dc * 128:(dc + 1) * 128], xg_[:, dc * 128:(dc + 1) * 128], ident_bf[:]))
                            for dc in range(8)], reads=[xg_.res, ident_bf.res], writes=[bk.res])
                        DVE.op(lambda h, bkb=bkb, xT_=xT_, st=st: h.tensor_copy(
                            out=xT_[:, :, st * 128:(st + 1) * 128], in_=bkb.rearrange("p (c t) -> p c t", c=8)),
                            reads=[bk.res], writes=[xT_.res])
                    if e + 1 < NE:
                        gather(e + 1)
                    for fg in range(4):
                        j = e * 4 + fg
                        ensure_gu(j + NGU - 1)
                        if fg == 1 and e > 0:
                            scatters(e - 1)
                        wg_, wu_ = wgb[j % NGU], wub[j % NGU]
                        for fb in range(4):
                            fi = fg * 4 + fb
                            fs = slice(fb * 128, (fb + 1) * 128)
                            bg, bu = banks[2 + (fi % 2) * 2], banks[3 + (fi % 2) * 2]
                            sl_ = silu[fi % 2]
                            PE.op(mm_group(bg[:], [(wg_[:, dc, fs], xT_[:, dc, :]) for dc in range(8)]),
                                  reads=[wg_.res, xT_.res], writes=[bg.res])
                            PE.op(mm_group(bu[:], [(wu_[:, dc, fs], xT_[:, dc, :]) for dc in range(8)]),
                                  reads=[wu_.res, xT_.res], writes=[bu.res])
                            ACT.op(lambda h, sl_=sl_, bg=bg: h.activation(out=sl_[:], in_=bg[:], func=AF.Silu),
                                   reads=[bg.res], writes=[sl_.res])
                            DVE.op(lambda h, sl_=sl_, bu=bu, fi=fi: h.tensor_tensor(out=hT_[:, fi, :], in0=bu[:], in1=sl_[:], op=ALU.mult),
                                   reads=[bu.res, sl_.res], writes=[hT_.res])
                    for dh in range(2):
                        wd = wdb[dh]
                        for st in range(4):
                            sq_, hf = st // 2, st % 2
                            row = sq_ * NE + e
                            ye_ = ye[st]
                            bk = banks[6 + (st % 2)]
                            PE.op(mm_group(bk[:], [(hT_[:, fi, st * 128:(st + 1) * 128], wd[:, fi, :])
                                                   for fi in range(16)]), reads=[hT_.res, wd.res], writes=[bk.res])
                            if st % 2 == 0:
                                ACT.op(lambda h, bk=bk, ye_=ye_, dh=dh, hf=hf, row=row: h.activation(
                                    out=ye_[:, dh * 512:(dh + 1) * 512], in_=bk[:], func=AF.Copy, scale=valT[:, hf, row:row + 1]),
                                    reads=[bk.res, valT.res], writes=[ye_.res])
                            else:
                                DVE.op(lambda h, bk=bk, ye_=ye_, dh=dh, hf=hf, row=row: h.tensor_scalar(
                                    out=ye_[:, dh * 512:(dh + 1) * 512], in0=bk[:], scalar1=valT[:, hf, row:row + 1], scalar2=None,
                                    op0=ALU.mult), reads=[bk.res, valT.res], writes=[ye_.res])
                        if e + 1 < NE:
                            ld_down(e + 1, dh)
                scatters(NE - 1)
                barrier()
        barrier()
    return nc, dbg


def _rel_bucket_np(rel):
    half, max_exact = 16, 8
    ret = np.where(rel > 0, half, 0)
    n = np.abs(rel)
    nf = np.maximum(n, max_exact).astype(np.float32)
    lg = (np.log(nf / np.float32(max_exact)) / np.float32(math.log(128 / max_exact)) * np.float32(half - max_exact))
    large = max_exact + lg.astype(np.int32)
    large = np.minimum(large, half - 1)
    return ret + np.where(n < max_exact, n, large)


def _host_inputs(inputs):
    f = lambda k: np.ascontiguousarray(np.asarray(inputs[k], dtype=np.float32))
    rel_bias = f("rel_bias")
    kl = np.arange(128)[:, None]
    u = np.arange(STRIP_W)[None, :] - 512
    bidx = _rel_bucket_np(kl - u)
    strip = np.ascontiguousarray(np.transpose(rel_bias[bidx], (2, 0, 1)))
    far = np.stack([rel_bias[31, :], rel_bias[15, :]], axis=1).reshape(1, NH * 2)
    far = np.ascontiguousarray(np.broadcast_to(far, (128, NH * 2)))
    lam4 = np.stack([f("lam_q1")[0], f("lam_k1")[0], f("lam_q2")[0], f("lam_k2")[0]], axis=0)
    pc = lambda a: np.ascontiguousarray(a.reshape(8, 128).T)
    rep2 = lambda a: np.ascontiguousarray(np.concatenate([a, a]).reshape(128, 1))
    conv_w = f("conv_w")[0]
    conv_w_l = np.ascontiguousarray(np.transpose(conv_w.reshape(4, 8, 128), (2, 1, 0)))
    grb, gib = f("gate_r_b")[0], f("gate_i_b")[0]
    gate_b = np.stack([np.stack([pc(grb[0]), pc(grb[1])], axis=1), np.stack([pc(gib[0]), pc(gib[1])], axis=1)], axis=1)
    lam_l = f("lru_lambda")[0]
    lam_l = np.stack([pc(lam_l[0]), pc(lam_l[1])], axis=1)
    w_router_l = np.ascontiguousarray(np.transpose(f("w_router")[0].reshape(8, 128, NE), (1, 0, 2)))
    shared = {
        "g_mix": f("g_mix")[0], "w_in": f("w_in")[0], "g_q": rep2(f("g_q")[0]), "g_k": rep2(f("g_k")[0]),
        "lam4": np.ascontiguousarray(lam4), "g_subln": np.ascontiguousarray(f("g_subln")[0].reshape(128, 1)),
        "bias_strip": strip, "bias_far": far,
        "conv_w": conv_w_l, "conv_b": pc(f("conv_b")[0]), "gate_r_w": f("gate_r_w")[0], "gate_b": np.ascontiguousarray(gate_b),
        "gate_i_w": f("gate_i_w")[0], "lru_lambda": np.ascontiguousarray(lam_l),
        "w_proj_attn": f("w_proj_attn")[0], "w_proj_lru": f("w_proj_lru")[0], "w_out": f("w_out")[0],
        "g_ffn": f("g_ffn")[0], "w_router": w_router_l, "w_gate_e": f("w_gate_e")[0],
        "w_up_e": f("w_up_e")[0], "w_down_e": f("w_down_e")[0],
    }
    x = f("x")
    in_maps = []
    for c in range(NCORES):
        m = dict(shared)
        m["x"] = np.ascontiguousarray(x[c * NSEQ:(c + 1) * NSEQ])
        in_maps.append(m)
    return in_maps


def kernel(**inputs):
    in_maps = _host_inputs(inputs)
    nc, _ = build_program()
    res = run_bass_kernel_spmd(nc, in_maps, core_ids=list(range(NCORES)))
    out = np.concatenate([np.asarray(r["out"]) for r in res.results], axis=0)
    return out.astype(np.float32)
```
